# Optimizing a Trainium2 kernel written in Bass

```python
import math, functools
import jax, jax.numpy as jnp
from jax import lax
import numpy as np

D_MODEL = 1024
BATCH = 16
SEQ = 2048
DEPTH = 1
DEC_BATCH = 32
DEC_SEQ = 8
PAST_LEN = 16384
PAGE_SIZE = 128

P_DIM = 256
H_A = 4
DH_A = 64
E_A = 2 * DH_A
H_R = 4
K_R = 128
V_R = 128
HGRN_CHUNK = 32
Q_BLOCK = 128
N_GROUPS = 4
EXPERTS_PER_GROUP = 4
N_EXPERTS = N_GROUPS * EXPERTS_PER_GROUP
TOP_K = 2
D_EXPERT = 256
ROPE_THETA = 10000.0
EPS = 1e-6
NEG_INF = -1e30
ATT_WIDTH = H_A * E_A
REC_WIDTH = H_R * V_R
IN_WIDTHS = (2 * H_A * DH_A, 2 * H_A * DH_A, H_A * E_A, H_R * K_R, H_R * K_R, H_R * V_R, H_R * V_R, D_MODEL, D_MODEL)
D_IN = sum(IN_WIDTHS)

kernel_name = 'diffattn_hgrn2_hmoe_hybrid_step'


def _rmsnorm(x, g):
    xf = x.astype(jnp.float32)
    y = xf * lax.rsqrt(jnp.mean(xf * xf, axis=-1, keepdims=True) + EPS) * g.astype(jnp.float32)
    return y.astype(x.dtype)


def _rope(x, pos):
    half = DH_A // 2
    inv_freq = ROPE_THETA ** (-jnp.arange(half, dtype=jnp.float32) / half)
    ang = pos.astype(jnp.float32)[:, None] * inv_freq[None, :]
    cos = jnp.cos(ang)[:, None, None, :]
    sin = jnp.sin(ang)[:, None, None, :]
    xf = x.astype(jnp.float32)
    x1, x2 = xf[..., :half], xf[..., half:]
    return jnp.concatenate([x1 * cos - x2 * sin, x2 * cos + x1 * sin], axis=-1).astype(x.dtype)


def _online_softmax_update(carry, s, v):
    m, l, acc = carry
    m_new = jnp.maximum(m, s.max(-1))
    alpha = jnp.exp(m - m_new)
    p = jnp.exp(s - m_new[..., None])
    l = l * alpha + p.sum(-1)
    acc = acc * alpha[..., None] + jnp.einsum('bhcts,bshe->bhcte', p, v.astype(jnp.float32))
    return m_new, l, acc


def _attend_prompt(q, k, v):
    B, S = q.shape[:2]
    nb = S // Q_BLOCK
    scale = DH_A ** -0.5
    qb = q.reshape(B, nb, Q_BLOCK, H_A, 2, DH_A).transpose(1, 0, 2, 3, 4, 5)
    key_pos = jnp.arange(S)
    vf = v.astype(jnp.float32)

    def block(args):
        qi, start = args
        s = jnp.einsum('bthcd,bshcd->bhcts', qi, k).astype(jnp.float32) * scale
        q_pos = start + jnp.arange(Q_BLOCK)
        s = jnp.where(key_pos[None, :] <= q_pos[:, None], s, NEG_INF)
        p = jax.nn.softmax(s, axis=-1)
        return jnp.einsum('bhcts,bshe->bhcte', p, vf)

    o = lax.map(block, (qb, jnp.arange(nb) * Q_BLOCK))
    return o.transpose(1, 2, 3, 0, 4, 5).reshape(B, H_A, 2, S, E_A)


def _attend_sample(q, k_new, v_new, cache_k, cache_v, page_table, layer):
    B, T = q.shape[:2]
    scale = DH_A ** -0.5
    m0 = jnp.full((B, H_A, 2, T), NEG_INF, jnp.float32)
    l0 = jnp.zeros((B, H_A, 2, T), jnp.float32)
    a0 = jnp.zeros((B, H_A, 2, T, E_A), jnp.float32)

    def page_step(carry, pages):
        kp = cache_k[layer, pages]
        vp = cache_v[layer, pages]
        s = jnp.einsum('bthcd,bshcd->bhcts', q, kp).astype(jnp.float32) * scale
        return _online_softmax_update(carry, s, vp), None

    carry, _ = lax.scan(page_step, (m0, l0, a0), page_table.T)
    s = jnp.einsum('bthcd,bshcd->bhcts', q, k_new).astype(jnp.float32) * scale
    s = jnp.where(jnp.tril(jnp.ones((T, T), bool)), s, NEG_INF)
    m, l, acc = _online_softmax_update(carry, s, v_new)
    return acc / l[..., None]


def _hgrn2(q, f_logit, v, lb, s0):
    B, T = q.shape[:2]
    C = math.gcd(T, HGRN_CHUNK)
    n = T // C
    f = lb + (1.0 - lb) * jax.nn.sigmoid(f_logit.astype(jnp.float32))
    g = jnp.log(f)
    k = 1.0 - f

    def chunks(a):
        return a.astype(jnp.float32).reshape(B, n, C, H_R, a.shape[-1]).transpose(1, 0, 3, 2, 4)

    tril = jnp.tril(jnp.ones((C, C), bool))

    def step(S, xs):
        qc, kc, vc, gc = xs
        b = jnp.cumsum(gc, axis=2)
        qe = qc * jnp.exp(b)
        A = jnp.where(tril, jnp.einsum('bhtk,bhsk->bhts', qe, kc * jnp.exp(-b)), 0.0)
        o = jnp.einsum('bhts,bhsv->bhtv', A, vc) + jnp.einsum('bhtk,bhkv->bhtv', qe, S)
        b_last = b[:, :, -1:, :]
        S = jnp.exp(b_last[:, :, 0, :])[..., None] * S + jnp.einsum('bhsk,bhsv->bhkv', kc * jnp.exp(b_last - b), vc)
        return S, o

    S, o = lax.scan(step, s0.astype(jnp.float32), (chunks(q), chunks(k), chunks(v), chunks(g)))
    return o.transpose(1, 0, 3, 2, 4).reshape(B, T, H_R, V_R), S


def _hier_moe(h, w_rg, b_rg, w_re, b_re, w_gate, w_up, w_down):
    pg = jax.nn.softmax((h @ w_rg + b_rg).astype(jnp.float32), axis=-1)
    p_top, g_idx = lax.top_k(pg, 1)
    le = (h @ w_re + b_re).astype(jnp.float32).reshape(-1, N_GROUPS, EXPERTS_PER_GROUP)
    le = jnp.einsum('ng,nge->ne', jax.nn.one_hot(g_idx[:, 0], N_GROUPS, dtype=jnp.float32), le)
    v2, e_idx = lax.top_k(le, TOP_K)
    w2 = jax.nn.softmax(v2, axis=-1) * p_top
    expert_id = g_idx * EXPERTS_PER_GROUP + e_idx
    combine = jnp.einsum('nk,nke->ne', w2, jax.nn.one_hot(expert_id, N_EXPERTS, dtype=jnp.float32)).astype(h.dtype)
    y = jnp.zeros_like(h)
    for grp in range(N_GROUPS):
        sl = slice(grp * EXPERTS_PER_GROUP, (grp + 1) * EXPERTS_PER_GROUP)
        a = jnp.einsum('nd,edf->nef', h, w_gate[sl])
        u = jnp.einsum('nd,edf->nef', h, w_up[sl])
        hid = jax.nn.silu(a) * u * combine[:, sl, None]
        y = y + jnp.einsum('nef,efd->nd', hid, w_down[sl])
    return y


def _decoder_layer(i, x, p, pos, attend, s0, g_mix, w_in, lam, g_subln, lb_param, g_rec,
                   w_branch_a, w_branch_r, w_out, g_ffn, w_route_group, b_route_group,
                   w_route_expert, b_route_expert, w_exp_gate, w_exp_up, w_exp_down,
                   g_ple, w_ple_gate, w_ple):
    B, T, _ = x.shape
    h = _rmsnorm(x, g_mix[i])
    proj = h @ w_in[i]
    q_a, k_a, v_a, q_r, f_r, i_r, g_r, gate_a, gate_r = jnp.split(proj, list(np.cumsum(IN_WIDTHS)[:-1]), axis=-1)

    q_a = _rope(q_a.reshape(B, T, H_A, 2, DH_A), pos)
    k_a = _rope(k_a.reshape(B, T, H_A, 2, DH_A), pos)
    v_a = v_a.reshape(B, T, H_A, E_A)
    o2 = attend(q_a, k_a, v_a)
    lam_init = 0.8 - 0.6 * math.exp(-0.3 * i)
    lamf = lam[i].astype(jnp.float32)
    lam_val = jnp.exp(jnp.sum(lamf[0] * lamf[1])) - jnp.exp(jnp.sum(lamf[2] * lamf[3])) + lam_init
    od = (o2[:, :, 0] - lam_val * o2[:, :, 1]).transpose(0, 2, 1, 3)
    y_a = (_rmsnorm(od, g_subln[i]) * (1.0 - lam_init)).reshape(B, T, ATT_WIDTH).astype(x.dtype)

    lb = jnp.cumsum(jax.nn.softmax(lb_param.astype(jnp.float32), axis=0), axis=0)[i].reshape(H_R, K_R)
    o_r, s_new = _hgrn2(jax.nn.silu(q_r).reshape(B, T, H_R, K_R), f_r.reshape(B, T, H_R, K_R),
                        i_r.reshape(B, T, H_R, V_R), lb, s0)
    y_r = _rmsnorm(o_r, g_rec[i]) * jax.nn.silu(g_r.reshape(B, T, H_R, V_R).astype(jnp.float32))
    y_r = y_r.reshape(B, T, REC_WIDTH).astype(x.dtype)

    merged = jax.nn.sigmoid(gate_a) * (y_a @ w_branch_a[i]) + jax.nn.sigmoid(gate_r) * (y_r @ w_branch_r[i])
    x = x + merged @ w_out[i]

    hf = _rmsnorm(x, g_ffn[i]).reshape(B * T, D_MODEL)
    x = x + _hier_moe(hf, w_route_group[i], b_route_group[i], w_route_expert[i], b_route_expert[i],
                      w_exp_gate[i], w_exp_up[i], w_exp_down[i]).reshape(B, T, D_MODEL)

    hp = _rmsnorm(x, g_ple[i])
    x = x + jax.nn.sigmoid(hp @ w_ple_gate[i]) * (p[i] @ w_ple[i])
    return x, k_a, v_a, s_new


def setup_inputs(seed: int = 0) -> dict:
    key = jax.random.key(seed)
    ks = jax.random.split(key, 32)
    n_pages = PAST_LEN // PAGE_SIZE
    n_used = DEC_BATCH * n_pages
    n_phys = n_used + (n_used + 3) // 4
    f32 = jnp.float32

    def nrm(k, shape, scale):
        return jax.random.normal(k, shape, f32) * scale

    def gain(k, shape):
        return 1.0 + 0.02 * jax.random.normal(k, shape, f32)

    perm = jax.random.permutation(ks[7], n_phys)[:n_used]
    return {
        'x_prompt': nrm(ks[0], (BATCH, SEQ, D_MODEL), 1.0),
        'x_sample': nrm(ks[1], (DEC_BATCH, DEC_SEQ, D_MODEL), 1.0),
        'p_prompt': nrm(ks[2], (DEPTH, BATCH, SEQ, P_DIM), 1.0),
        'p_sample': nrm(ks[3], (DEPTH, DEC_BATCH, DEC_SEQ, P_DIM), 1.0),
        'cache_k': nrm(ks[4], (DEPTH, n_phys, PAGE_SIZE, H_A, 2, DH_A), 1.0),
        'cache_v': nrm(ks[5], (DEPTH, n_phys, PAGE_SIZE, H_A, E_A), 1.0),
        'state_hgrn': nrm(ks[6], (DEPTH, DEC_BATCH, H_R, K_R, V_R), 0.5),
        'page_table': perm.reshape(DEC_BATCH, n_pages).astype(jnp.int32),
        'g_mix': gain(ks[8], (DEPTH, D_MODEL)),
        'w_in': nrm(ks[9], (DEPTH, D_MODEL, D_IN), D_MODEL ** -0.5),
        'lam': nrm(ks[10], (DEPTH, 4, DH_A), 0.1),
        'g_subln': gain(ks[11], (DEPTH, E_A)),
        'lb_param': nrm(ks[12], (DEPTH + 1, H_R * K_R), 0.1),
        'g_rec': gain(ks[13], (DEPTH, V_R)),
        'w_branch_a': nrm(ks[14], (DEPTH, ATT_WIDTH, D_MODEL), ATT_WIDTH ** -0.5),
        'w_branch_r': nrm(ks[15], (DEPTH, REC_WIDTH, D_MODEL), REC_WIDTH ** -0.5),
        'w_out': nrm(ks[16], (DEPTH, D_MODEL, D_MODEL), D_MODEL ** -0.5),
        'g_ffn': gain(ks[17], (DEPTH, D_MODEL)),
        'w_route_group': nrm(ks[18], (DEPTH, D_MODEL, N_GROUPS), D_MODEL ** -0.5),
        'b_route_group': nrm(ks[19], (DEPTH, N_GROUPS), 0.01),
        'w_route_expert': nrm(ks[20], (DEPTH, D_MODEL, N_EXPERTS), D_MODEL ** -0.5),
        'b_route_expert': nrm(ks[21], (DEPTH, N_EXPERTS), 0.01),
        'w_exp_gate': nrm(ks[22], (DEPTH, N_EXPERTS, D_MODEL, D_EXPERT), D_MODEL ** -0.5),
        'w_exp_up': nrm(ks[23], (DEPTH, N_EXPERTS, D_MODEL, D_EXPERT), D_MODEL ** -0.5),
        'w_exp_down': nrm(ks[24], (DEPTH, N_EXPERTS, D_EXPERT, D_MODEL), D_EXPERT ** -0.5),
        'g_ple': gain(ks[25], (DEPTH, D_MODEL)),
        'w_ple_gate': nrm(ks[26], (DEPTH, D_MODEL, D_MODEL), D_MODEL ** -0.5),
        'w_ple': nrm(ks[27], (DEPTH, P_DIM, D_MODEL), P_DIM ** -0.5),
        'g_final': gain(ks[28], (D_MODEL,)),
    }


def reference(x_prompt, x_sample, p_prompt, p_sample, cache_k, cache_v, state_hgrn, page_table,
              g_mix, w_in, lam, g_subln, lb_param, g_rec, w_branch_a, w_branch_r, w_out, g_ffn,
              w_route_group, b_route_group, w_route_expert, b_route_expert, w_exp_gate, w_exp_up,
              w_exp_down, g_ple, w_ple_gate, w_ple, g_final):
    B, S = x_prompt.shape[:2]
    T = x_sample.shape[1]
    pos_p = jnp.arange(S, dtype=jnp.int32)
    pos_s = PAST_LEN + jnp.arange(T, dtype=jnp.int32)
    weights = (g_mix, w_in, lam, g_subln, lb_param, g_rec, w_branch_a, w_branch_r, w_out, g_ffn,
               w_route_group, b_route_group, w_route_expert, b_route_expert, w_exp_gate, w_exp_up,
               w_exp_down, g_ple, w_ple_gate, w_ple)
    s0_prompt = jnp.zeros((B, H_R, K_R, V_R), jnp.float32)
    xp, xs = x_prompt, x_sample
    kp_l, vp_l, sp_l, ks_l, vs_l, ss_l = [], [], [], [], [], []
    for i in range(DEPTH):
        attend_s = functools.partial(_attend_sample, cache_k=cache_k, cache_v=cache_v,
                                     page_table=page_table, layer=i)
        xp, kp, vp, sp = _decoder_layer(i, xp, p_prompt, pos_p, _attend_prompt, s0_prompt, *weights)
        xs, k_s, v_s, s_s = _decoder_layer(i, xs, p_sample, pos_s, attend_s, state_hgrn[i], *weights)
        kp_l.append(kp); vp_l.append(vp); sp_l.append(sp)
        ks_l.append(k_s); vs_l.append(v_s); ss_l.append(s_s)
    y_prompt = _rmsnorm(xp, g_final)
    y_sample = _rmsnorm(xs, g_final)
    return (y_prompt, y_sample, jnp.stack(kp_l), jnp.stack(vp_l), jnp.stack(sp_l),
            jnp.stack(ks_l), jnp.stack(vs_l), jnp.stack(ss_l))
```

```python
import contextlib
import os
DBG = set(os.environ.get("K_DBG", "").split(","))
import math
import numpy as np
import concourse.bass as bass
import concourse.mybir as mybir
from concourse.bass_utils import run_bass_kernel_spmd

F32 = mybir.dt.float32
BF16 = mybir.dt.bfloat16
I32 = mybir.dt.int32
AF = mybir.ActivationFunctionType
ALU = mybir.AluOpType

D = 1024; SEQ = 2048; NSEQ = 2; NSAMP = 4; TS = 8; PAST = 16384; PAGE = 128
DIN = 5632; NPHYS = 5120; NPAGES = 128
DO_SAMPLE = True; DO_PROMPT = True; STOP = 99
EPS = 1e-6
LAM_INIT = 0.8 - 0.6 * math.exp(-0.3 * 0)
OQ, OK_, OV, OQR, OFR, OIR, OGR, OGA, OGRR = 0, 512, 1024, 1536, 2048, 2560, 3072, 3584, 4608


def _region(ap):
    t = ap.tensor
    tn = type(t).__name__
    if tn.startswith("DRam"):
        return ("D:" + t.name, 0, 1, 0, 1)
    if tn.startswith("PSum"):
        return ("P:" + t.name, 0, 128, 0, 1)
    dims = list(ap.ap)
    esz = mybir.dt.size(ap.dtype)
    pstride = dims[0][0]
    off = ap.offset
    if pstride == 0:
        p0 = 0; npart = 1; f0 = off
    else:
        p0 = off // pstride; npart = dims[0][1]; f0 = off % pstride
    ext = 1
    for st, cnt in dims[1:]:
        ext += (cnt - 1) * abs(st)
    return ("S:" + t.name, p0, p0 + npart, f0 * esz, (f0 + ext) * esz)


class Op:
    __slots__ = ("eng", "fn", "deps", "inc", "idx", "dma")

    def __init__(self, eng, fn, dma):
        self.eng = eng; self.fn = fn; self.deps = set(); self.inc = False; self.idx = -1; self.dma = dma


class Prog:
    ENGS = ("pe", "dve", "act", "pool", "sp")

    def __init__(self, nc, n_dma_sems=24):
        self.nc = nc; self.ops = []; self.acc = {}; self.n_dma_sems = n_dma_sems

    def _add(self, eng, fn, reads, writes, dma=False):
        op = Op(eng, fn, dma)
        op.idx = len(self.ops)
        self.ops.append(op)
        for ap in reads:
            self._access(op, ap, False)
        for ap in writes:
            self._access(op, ap, True)
        best = {}
        keep = set()
        for d in op.deps:
            p = self.ops[d]
            if p.dma:
                keep.add(d)
            elif best.get(p.eng, -1) < d:
                best[p.eng] = d
        keep.update(best.values())
        op.deps = keep
        return op

    def _access(self, op, ap, is_write):
        key, p0, p1, f0, f1 = _region(ap)
        if key.startswith("D:") and not (key.startswith("D:scr") or key == "D:v_s"):
            return
        lst = self.acc.get(key)
        if lst is None:
            lst = self.acc[key] = []
        keep = []
        psum = key.startswith("P:")
        for rec in lst:
            q0, q1, g0, g1, oi, w = rec
            ov = not (q1 <= p0 or p1 <= q0 or g1 <= f0 or f1 <= g0)
            if ov and (is_write or w or (psum and self.ops[oi].eng != op.eng)) and oi != op.idx:
                op.deps.add(oi)
            if is_write and ov and q0 >= p0 and q1 <= p1 and g0 >= f0 and g1 <= f1:
                continue
            keep.append(rec)
        keep.append((p0, p1, f0, f1, op.idx, is_write))
        self.acc[key] = keep

    def op(self, eng, fn, reads=(), writes=()):
        return self._add(eng, fn, reads, writes, False)

    def dma(self, eng, out, in_, extra_reads=(), fn=None):
        if fn is None:
            fn = lambda e: e.dma_start(out=out, in_=in_)
        return self._add(eng, fn, [in_] + list(extra_reads), [out], True)

    def emit(self):
        nc = self.nc
        ops = self.ops
        engobj = {"pe": nc.tensor, "dve": nc.vector, "act": nc.scalar, "pool": nc.gpsimd, "sp": nc.sync}
        for op in ops:
            for d in op.deps:
                p = ops[d]
                if p.eng == "pe" and op.eng == "pe" and not p.dma:
                    continue
                p.inc = True
        with contextlib.ExitStack() as st:
            esem = {e: st.enter_context(nc.semaphore("sem_" + e)) for e in self.ENGS}
            nds = self.n_dma_sems
            dsem = [st.enter_context(nc.semaphore("dsem%d" % i)) for i in range(2 * nds)]
            ecount = {e: 0 for e in self.ENGS}
            dcount = [0] * (2 * nds)
            waited = {e: {} for e in self.ENGS}
            nd = 0
            ndq = {"sp": 0, "pool": 0, "act": 0}
            tok = {}

            def wait(eng, key, sem, val):
                w = waited[eng]
                if w.get(key, 0) >= val:
                    return
                engobj[eng].wait_ge(sem, val)
                w[key] = val

            for op in ops:
                e = op.eng
                for d in sorted(op.deps):
                    p = ops[d]
                    if p.eng == "pe" and e == "pe" and not p.dma:
                        continue
                    key, sem, val = tok[d]
                    wait(e, key, sem, val)
                if op.dma:
                    si = (ndq[e] % nds) + (nds if e == "pool" else 0)
                    ndq[e] += 1
                    nd += 1
                    if dcount[si]:
                        wait(e, "d%d" % si, dsem[si], dcount[si])
                    ins = op.fn(engobj[e])
                    dcount[si] += 16
                    ins.then_inc(dsem[si], 16)
                    tok[op.idx] = ("d%d" % si, dsem[si], dcount[si])
                else:
                    ins = op.fn(engobj[e])
                    if op.inc:
                        ecount[e] += 1
                        ins.then_inc(esem[e], 1)
                        tok[op.idx] = ("e" + e, esem[e], ecount[e])
            for si in range(2 * nds):
                if dcount[si]:
                    wait("sp", "d%d" % si, dsem[si], dcount[si])
            self.stats = dict(ecount=dict(ecount), ndma=nd, nops=len(ops))


def build_program():
    nc = bass.Bass("TRN2", target_bir_lowering=False)
    es = contextlib.ExitStack()

    def din(name, shape, dt=F32):
        return nc.dram_tensor(name, list(shape), dt, kind="ExternalInput").ap()

    def dout(name, shape, dt=F32):
        return nc.dram_tensor(name, list(shape), dt, kind="ExternalOutput").ap()

    x_p = din("x_p", [NSEQ * SEQ, D]); x_s = din("x_s", [NSAMP * TS, D])
    p_p = din("p_p", [NSEQ * SEQ, 256]); p_s = din("p_s", [NSAMP * TS, 256])
    cache_k = din("cache_k", [NPHYS * PAGE, 512]); cache_v = din("cache_v", [NPHYS * PAGE, 512])
    state0 = din("state0", [NSAMP, 4, 128, 128]); ptab = din("ptab", [NSAMP, NPAGES], I32)
    w_in = din("w_in", [D, DIN]); w_ba = din("w_ba", [512, D]); w_br = din("w_br", [512, D])
    w_out = din("w_out", [D, D]); w_g = din("w_g", [16, D, 256]); w_u = din("w_u", [16, D, 256])
    w_d = din("w_d", [16, 256, D]); w_pg = din("w_pg", [D, D]); w_ple = din("w_ple", [256, D])
    w_rt = din("w_rt", [D, 20]); b_rt = din("b_rt", [1, 20])
    gains = din("gains", [4, D])
    gsmall = din("gsmall", [2, 128])
    lam = din("lam", [1, 256]); lbp = din("lbp", [2, 512])
    c_ident = din("c_ident", [128, 128]); c_U = din("c_U", [128, 128]); c_U2 = din("c_U2", [128, 128])
    c_caus = din("c_caus", [128, 256])
    c_cos = din("c_cos", [SEQ + 128, 256]); c_sin = din("c_sin", [SEQ + 128, 256])
    c_cm = din("c_cm", [128, 8])
    c_sel = din("c_sel", [NSAMP, 64, 512]); c_hm = din("c_hm", [64, 8]); c_pos = din("c_pos", [128, 1])
    c_tri8 = din("c_tri8", [64, 8])

    y_p = dout("y_p", [NSEQ * SEQ, D]); y_s = dout("y_s", [NSAMP * TS, D])
    k_p = dout("k_p", [NSEQ * SEQ, 512]); v_p = dout("v_p", [NSEQ * SEQ, 512])
    s_p = dout("s_p", [NSEQ, 4, 128, 128]); k_s = dout("k_s", [NSAMP * TS, 512])
    v_s = dout("v_s", [NSAMP * TS, 512]); s_s = dout("s_s", [NSAMP, 4, 128, 128])

    def scr(name, shape):
        return nc.dram_tensor("scr_" + name, list(shape), BF16, kind="Internal").ap()

    scr_win = scr("win", [D, DIN]); scr_wba = scr("wba", [512, D]); scr_wbr = scr("wbr", [512, D])
    scr_wout = scr("wout", [D, D]); scr_wg = scr("wg", [16, D, 256]); scr_wu = scr("wu", [16, D, 256])
    scr_wd = scr("wd", [16, 256, D]); scr_wpg = scr("wpg", [D, D]); scr_wple = scr("wple", [256, D])

    def sb(name, shape, dt=F32):
        return es.enter_context(nc.sbuf_tensor(name, list(shape), dt))

    ring = sb("ring", [128, 4, 4096], BF16)
    proj = sb("proj", [128, 2, DIN])
    KTt = sb("KTt", [128, 4, 2048], BF16)
    Vpt = sb("Vpt", [128, 16, 4, 130], BF16)
    xt = sb("xt", [128, 2, D])
    xT = sb("xT", [128, 8, 256], BF16)
    hTf = sb("hTf", [128, 8, 128])
    wk = sb("wk", [128, 1024])
    wk2 = sb("wk2", [128, 1024])
    wkb = sb("wkb", [128, 1024], BF16)
    kout = sb("kout", [128, 512])
    qT = sb("qT", [128, 4, 128], BF16)
    qblk = sb("qblk", [128, 4, 2, 128], BF16)
    pT4 = sb("pT4", [128, 4, 512], BF16)
    od = sb("od", [128, 512])
    yaT2 = sb("yaT2", [128, 2, 4, 128], BF16); yrT2 = sb("yrT2", [128, 2, 4, 128], BF16)
    gbuf = sb("gbuf", [128, 512]); kbuf = sb("kbuf", [128, 512])
    kd = sb("kd", [128, 512], BF16); vb = sb("vb", [128, 512], BF16)
    vblk = sb("vblk", [128, 4, 4, 128], BF16)
    ebT = sb("ebT", [128, 512]); enbT = sb("enbT", [128, 512])
    dec = sb("dec", [128, 16])
    qeT = sb("qeT", [128, 4, 128], BF16); keT = sb("keT", [128, 4, 128], BF16)
    ATm = sb("ATm", [128, 4, 128], BF16)
    Sst = sb("Sst", [128, 2, 4, 128])
    Sb = sb("Sb", [128, 4, 4, 128], BF16)
    merged = sb("merged", [128, 1024], BF16)
    hid = sb("hid", [128, 2, 2, 256], BF16)
    cmb = sb("cmb", [128, 2, 16])
    rt = sb("rt", [128, 64])
    st4 = sb("st4", [128, 32])
    ptile = sb("ptile", [128, 256]); ppT = sb("ppT", [128, 2, 256], BF16)
    cosb = sb("cosb", [128, 256]); sinb = sb("sinb", [128, 256])
    ident = sb("ident", [128, 128]); identb = sb("identb", [128, 128], BF16)
    Um = sb("Um", [128, 128]); U2m = sb("U2m", [128, 128]);
    causb = sb("causb", [128, 256], BF16)
    cm = sb("cm", [128, 8])
    gbh = sb("gbh", [128, 3, D], BF16)
    gfin = sb("gfin", [128, D])
    gsub = sb("gsub", [128, 128]); grec4 = sb("grec4", [128, 4, 128])
    lamv = sb("lamv", [128, 4])
    lb_b = sb("lb_b", [128, 512]); oml_b = sb("oml_b", [128, 512])
    wrt = sb("wrt", [128, 8, 20]); brt = sb("brt", [128, 20])
    hm = sb("hm", [64, 8]); posf = sb("posf", [128, 1])
    tri8 = sb("tri8", [64, 8])
    idx = sb("idx", [128, NPAGES], I32)
    pti = kout[:, 256:256 + NPAGES].bitcast(I32)
    Zq = sb("Zq", [128, 4, 64], BF16)
    sacc = sb("sacc", [64, 128]); sml = sb("sml", [64, 16])
    onorm = sb("onorm", [64, 128])
    sqT = sb("sqT", [128, 4, 128], BF16); skT = sb("skT", [128, 4, 128], BF16)
    sod = sb("sod", [128, 512], BF16)

    ps = [es.enter_context(nc.psum_tensor("ps%d" % i, [128, 512], F32)) for i in range(8)]
    P = Prog(nc)
    cnt = {"bank": 0, "ring": 0, "ev": 0, "bset": 8, "sl": 0, "inter": False, "hb": 0, "ti": 0}

    def hbank():
        if cnt["inter"]:
            cnt["hb"] = (cnt["hb"] + 1) % 3
            return ps[cnt["hb"]]
        return gbank()

    def gbank():
        if cnt["inter"]:
            cnt["bank"] = (cnt["bank"] + 1) % 3
            return ps[3 + cnt["bank"]]
        cnt["bank"] = (cnt["bank"] + 1) % cnt["bset"]
        return ps[cnt["bank"]]

    def aps(*xs):
        return [x for x in xs if x is not None and not isinstance(x, (int, float))]

    def mm(out, lhsT, rhs, start, stop, **kw):
        P.op("pe", lambda e: e.matmul(out, lhsT=lhsT, rhs=rhs, start=start, stop=stop, **kw), [lhsT, rhs], [out])

    def tr(out, in_, idn):
        P.op("pe", lambda e: e.transpose(out=out, in_=in_, identity=idn), [in_, idn], [out])

    def act(out, in_, func, bias=None, scale=None, accum_out=None, eng="act"):
        kw = {}
        if bias is not None: kw["bias"] = bias
        if scale is not None: kw["scale"] = scale
        if accum_out is not None: kw["accum_out"] = accum_out
        P.op("act", lambda e: e.activation(out=out, in_=in_, func=func, **kw),
             aps(in_, bias, scale), aps(out, accum_out))

    def tt(out, in0, in1, op, eng="dve"):
        P.op(eng, lambda e: e.tensor_tensor(out=out, in0=in0, in1=in1, op=op), [in0, in1], [out])

    def ts(out, in0, s1, s2, op0, op1=None, eng="dve"):
        if op1 is None:
            P.op(eng, lambda e: e.tensor_scalar(out=out, in0=in0, scalar1=s1, scalar2=None, op0=op0), aps(in0, s1), [out])
        else:
            P.op(eng, lambda e: e.tensor_scalar(out=out, in0=in0, scalar1=s1, scalar2=s2, op0=op0, op1=op1),
                 aps(in0, s1, s2), [out])

    def stt(out, in0, scalar, in1, op0, op1, eng="dve"):
        P.op(eng, lambda e: e.scalar_tensor_tensor(out=out, in0=in0, scalar=scalar, in1=in1, op0=op0, op1=op1),
             aps(in0, scalar, in1), [out])

    def cp(out, in_, eng=None):
        if eng is None:
            cnt["ev"] += 1
            eng = "act" if cnt["ev"] % 2 else "dve"
        if eng == "act":
            P.op("act", lambda e: e.copy(out=out, in_=in_), [in_], [out])
        else:
            P.op(eng, lambda e: e.tensor_copy(out=out, in_=in_), [in_], [out])

    def recip(out, in_):
        P.op("dve", lambda e: e.reciprocal(out=out, in_=in_), [in_], [out])

    def memset(ap, v, eng="dve"):
        P.op(eng, lambda e: e.memset(ap, v), [], [ap])

    def rmax(out, in_):
        P.op("dve", lambda e: e.reduce_max(out=out, in_=in_, axis=mybir.AxisListType.X), [in_], [out])

    def rsum(out, in_):
        P.op("dve", lambda e: e.reduce_sum(out=out, in_=in_, axis=mybir.AxisListType.X), [in_], [out])

    def load(out, in_, eng="sp"):
        P.dma(eng, out, in_)

    def store(out, in_, eng="pool"):
        P.dma(eng, out, in_)

    def wslot():
        cnt["ring"] = (cnt["ring"] + 1) % 4
        return ring[:, cnt["ring"], :]

    def rstd_of(src, n, dst):
        act(wk2[:, 0:n], src, AF.Square, accum_out=st4[:, 15:16])
        act(st4[:, 14:15], st4[:, 15:16], AF.Sqrt, bias=EPS, scale=1.0 / n)
        recip(dst, st4[:, 14:15])

    load(ident[:], c_ident); load(Um[:], c_U); load(U2m[:], c_U2); load(wk2[:, 0:256], c_caus); load(cm[:], c_cm)
    cp(identb[:], ident[:], "dve"); cp(causb[:], wk2[:, 0:256], "dve")
    for i in range(3):
        load(wk[:], gains[i].partition_broadcast(128))
        cp(gbh[:, i, :], wk[:], "dve")
    load(gfin[:], gains[3].partition_broadcast(128))
    load(gsub[:], gsmall[0].partition_broadcast(128))
    for h in range(4):
        load(grec4[:, h, :], gsmall[1].partition_broadcast(128))
    ts(gsub[:], gsub[:], float((1.0 - LAM_INIT) * math.sqrt(1.0)), None, ALU.mult)
    lamb = wk2[:, 256:512]
    load(lamb, lam[0].partition_broadcast(128))
    lbb = wk2[:, 0:1024].rearrange("p (a b) -> p a b", a=2)
    load(wrt[:], w_rt.rearrange("(c p) n -> p c n", p=128)); load(brt[:], b_rt[0].partition_broadcast(128))
    load(hm[:], c_hm); load(posf[:], c_pos); load(tri8[:], c_tri8)
    tt(wk[:, 0:64], lamb[:, 0:64], lamb[:, 64:128], ALU.mult)
    rsum(lamv[:, 2:3], wk[:, 0:64])
    tt(wk[:, 64:128], lamb[:, 128:192], lamb[:, 192:256], ALU.mult)
    rsum(lamv[:, 3:4], wk[:, 64:128])
    act(lamv[:, 2:4], lamv[:, 2:4], AF.Exp)
    tt(lamv[:, 0:1], lamv[:, 2:3], lamv[:, 3:4], ALU.subtract)
    ts(lamv[:, 0:1], lamv[:, 0:1], float(LAM_INIT), None, ALU.add)
    ts(lamv[:, 1:2], lamv[:, 0:1], -1.0, None, ALU.mult)
    load(lbb[:, 0, :], lbp[0].partition_broadcast(128)); load(lbb[:, 1, :], lbp[1].partition_broadcast(128))
    tt(lb_b[:], lbb[:, 1, :], lbb[:, 0, :], ALU.subtract)
    act(lb_b[:], lb_b[:], AF.Exp)
    ts(lb_b[:], lb_b[:], 1.0, None, ALU.add)
    recip(lb_b[:], lb_b[:])
    ts(oml_b[:], lb_b[:], -1.0, 1.0, ALU.mult, ALU.add)

    def cast_rows(src, dst, rows):
        n = src.shape[0]
        for r0 in range(0, n, rows):
            P.dma("pool", dst[r0:r0 + rows], src[r0:r0 + rows])

    if STOP >= 1:
        cast_rows(w_in, scr_win, 128)
        cast_rows(w_ba, scr_wba, 512); cast_rows(w_br, scr_wbr, 512); cast_rows(w_out, scr_wout, 512)
        for e in range(16):
            P.dma("pool", scr_wg[e], w_g[e]); P.dma("pool", scr_wu[e], w_u[e]); P.dma("pool", scr_wd[e], w_d[e])
        cast_rows(w_pg, scr_wpg, 512); cast_rows(w_ple, scr_wple, 256)

    KT = KTt[:, :, 0:SEQ]
    hwk = hTf[:].rearrange("p c t -> p (c t)")
    hwkb = merged
    memset(qblk[:], 0.0)
    Vp = Vpt[:, 0:SEQ // 128]
    scale = 0.125

    def stage_in(ti, xrows, prow, sample):
        if sample:
            memset(xt[:, ti, :], 0.0)
            memset(ptile[:], 0.0)
            for b in range(NSAMP):
                load(xt[32 * b:32 * b + TS, ti, :], x_s[b * TS:(b + 1) * TS, :])
        else:
            load(xt[:, ti, :], x_p[xrows:xrows + 128, :])
        rstd_of(xt[:, ti, :], D, st4[:, 0:1])
        stt(wkb[:], xt[:, ti, :], st4[:, 0:1], gbh[:, 0, :], ALU.mult, ALU.mult)
        bank = gbank()
        pb = bank[:].bitcast(BF16)
        for c in range(8):
            tr(pb[:, c * 128:(c + 1) * 128], wkb[:, c * 128:(c + 1) * 128], identb[:])
        cp(xT[:, :, ti * 128:(ti + 1) * 128], pb.rearrange("p (c t) -> p c t", c=8))

    def stage_proj(ntile, jmax=11):
        wv = scr_win.rearrange("(c p) n -> p c n", p=128)
        for j in range(jmax):
            slot = wslot().rearrange("p (c n) -> p c n", c=8)
            load(slot, wv[:, :, j * 512:(j + 1) * 512])
            for ti in range(ntile):
                bank = gbank()
                for c in range(8):
                    mm(bank[:], xT[:, c, ti * 128:(ti + 1) * 128], slot[:, c, :], c == 0, c == 7)
                cp(proj[:, ti, j * 512:(j + 1) * 512], bank[:])

    def rope(dst, src, cs, sn):
        s4 = src.rearrange("p (g two d) -> p g two d", g=8, two=2)
        d4 = dst.rearrange("p (g two d) -> p g two d", g=8, two=2)
        c3 = cs.rearrange("p (g d) -> p g d", g=8); s3 = sn.rearrange("p (g d) -> p g d", g=8)
        t1 = wk[:, 0:256].rearrange("p (g d) -> p g d", g=8)
        t2 = wk[:, 256:512].rearrange("p (g d) -> p g d", g=8)
        t3 = wk[:, 512:768].rearrange("p (g d) -> p g d", g=8)
        t4 = wk[:, 768:1024].rearrange("p (g d) -> p g d", g=8)
        tt(t1, s4[:, :, 0, :], c3, ALU.mult)
        tt(t2, s4[:, :, 1, :], s3, ALU.mult)
        tt(t3, s4[:, :, 1, :], c3, ALU.mult)
        tt(t4, s4[:, :, 0, :], s3, ALU.mult)
        tt(d4[:, :, 0, :], t1, t2, ALU.subtract)
        tt(d4[:, :, 1, :], t3, t4, ALU.add)

    def stage_qkv(ti, orow, posrow, tseq, sample):
        load(cosb[:], c_cos[posrow:posrow + 128, :]); load(sinb[:], c_sin[posrow:posrow + 128, :])
        rope(kout[:], proj[:, ti, OK_:OK_ + 512], cosb[:], sinb[:])
        if sample:
            for b in range(NSAMP):
                store(k_s[b * TS:(b + 1) * TS, :], kout[32 * b:32 * b + TS, :])
                store(v_s[b * TS:(b + 1) * TS, :], proj[32 * b:32 * b + TS, ti, OV:OV + 512])
        else:
            store(k_p[orow:orow + 128, :], kout[:])
            store(v_p[orow:orow + 128, :], proj[:, ti, OV:OV + 512])
        rope(wk2[:, 0:512], proj[:, ti, OQ:OQ + 512], cosb[:], sinb[:])
        cp(wkb[:, 0:512], wk2[:, 0:512]); cp(wkb[:, 512:1024], kout[:])
        bank = gbank(); pb = bank[:].bitcast(BF16)
        for h in range(8):
            tr(pb[:, h * 128:(h + 1) * 128], wkb[:, h * 128:(h + 1) * 128], identb[:])
        cp(qT[:].rearrange("p h t -> p (h t)"), pb[:, 0:512], "act")
        if not sample:
            cp(qblk[0:64, :, 0, :], pb[0:64, 0:512].rearrange("p (h t) -> p h t", h=4), "act")
            cp(qblk[64:128, :, 1, :], pb[64:128, 0:512].rearrange("p (h t) -> p h t", h=4), "act")
        if sample:
            cp(keT[:].rearrange("p h t -> p (h t)"), pb[:, 512:1024])
            return
        if "noKT" not in DBG:
            if "ktalt" in DBG:
                for h in range(4):
                    cp(merged[:, h * 128:(h + 1) * 128], pb[:, (4 + h) * 128:(5 + h) * 128], "act")
            elif "kt2d" in DBG:
                for h in range(4):
                    cp(KT[:, h, tseq * 128:(tseq + 1) * 128], pb[:, (4 + h) * 128:(5 + h) * 128], "act")
            elif "ktlow" in DBG:
                cp(KT[:, :, tseq * 128:(tseq + 1) * 128], pb[:, 0:512].rearrange("p (h t) -> p h t", h=4), "act")
            else:
                cp(KT[:, :, tseq * 128:(tseq + 1) * 128], pb[:, 512:1024].rearrange("p (h t) -> p h t", h=4),
                   "dve" if "ktdve" in DBG else ("act" if "ktact" in DBG else None))
        if "noVp" not in DBG:
            cp(Vp[:, tseq, :, 0:128], proj[:, ti, OV:OV + 512].rearrange("p (h e) -> p h e", h=4),
               "dve" if "vpdve" in DBG else "act")

    def attn_finish(odv_src0, odv_src1, h, rec2):
        ts(st4[:, 4:5], rec2[1], lamv[:, 1:2], None, ALU.mult)
        ts(wk[:, 0:128], odv_src0, rec2[0], None, ALU.mult)
        stt(od[:, h * 128:(h + 1) * 128], odv_src1, st4[:, 4:5], wk[:, 0:128], ALU.mult, ALU.add)

    def subln_to_yaT():
        for h in range(4):
            act(wk2[:, 0:128], od[:, h * 128:(h + 1) * 128], AF.Square, accum_out=st4[:, 8 + h:9 + h])
        act(st4[:, 8:12], st4[:, 8:12], AF.Sqrt, bias=EPS, scale=1.0 / 128)
        recip(st4[:, 8:12], st4[:, 8:12])
        for h in range(4):
            stt(wkb[:, h * 128:(h + 1) * 128], od[:, h * 128:(h + 1) * 128], st4[:, 8 + h:9 + h], gsub[:], ALU.mult, ALU.mult)
        bank = gbank(); pb = bank[:].bitcast(BF16)
        for h in range(4):
            tr(pb[:, h * 128:(h + 1) * 128], wkb[:, h * 128:(h + 1) * 128], identb[:])
        cp(yaT2[:, cnt["ti"], :, :].rearrange("p h t -> p (h t)"), pb[:, 0:512])

    def stage_attn_prompt(tseq):
        cnt["bset"] = 2
        nk = tseq + 1
        steps = []
        for h in range(4):
            i = 0
            while i < nk:
                n = 2 if i + 1 < nk else 1
                steps.append((h, i, n)); i += n
        if cnt["inter"]:
            sbanks = [ps[3], ps[4], ps[5]]
            accs = [(ps[6], ps[7]), (ps[6], ps[7])]
        else:
            sbanks = [ps[2], ps[3], ps[4], ps[5]]
            accs = [(ps[6], ps[7]), (ps[0], ps[1])]
        nsb = len(sbanks)
        LA = 2

        def S_E(k):
            h, i0, n = steps[k]
            bank = sbanks[k % nsb]
            for u in range(n):
                mm(bank[:, u * 256:(u + 1) * 256], KT[:, h, (i0 + u) * 128:(i0 + u + 1) * 128],
                   qblk[:, h, :, :].rearrange("p c t -> p (c t)"), True, True)
            pt = pT4[:, k % 4, 0:n * 256]
            act(pt, bank[:, 0:n * 256], AF.Exp, scale=scale)
            if i0 + n - 1 == tseq:
                pd = pT4[:, k % 4, (n - 1) * 256:n * 256]
                tt(pd, pd, causb[:], ALU.mult)

        def PV(k):
            h, i0, n = steps[k]
            a0, a1 = accs[h % 2]
            for u in range(n):
                i = i0 + u
                for c, ab in enumerate((a0, a1)):
                    mm(ab[:, 0:130], pT4[:, k % 4, u * 256 + c * 128:u * 256 + (c + 1) * 128], Vp[:, i, h, :],
                       i == 0, i == tseq)
            if i0 + n - 1 == tseq:
                recip(st4[:, 5:6], a0[:, 128:129]); recip(st4[:, 6:7], a1[:, 128:129])
                attn_finish(a0[:, 0:128], a1[:, 0:128], h, (st4[:, 5:6], st4[:, 6:7]))

        for k in range(len(steps) + LA):
            if k < len(steps):
                S_E(k)
            if k - LA >= 0:
                PV(k - LA)
            yield
        subln_to_yaT()
        cnt["bset"] = 8
        yield

    def stage_hgrn(ti, sample, first, last, seq):
        pj = proj[:, ti, :]
        act(hwk[:, 0:512], pj[:, OFR:OFR + 512], AF.Sigmoid)
        tt(hwk[:, 0:512], hwk[:, 0:512], oml_b[:], ALU.mult)
        tt(hwk[:, 0:512], hwk[:, 0:512], lb_b[:], ALU.add)
        act(gbuf[:], hwk[:, 0:512], AF.Ln)
        ts(kbuf[:], hwk[:, 0:512], -1.0, 1.0, ALU.mult, ALU.add)
        yield
        if sample:
            ts(gbuf[:], gbuf[:], cm[:, 4:5], None, ALU.mult)
            ts(kbuf[:], kbuf[:], cm[:, 4:5], None, ALU.mult)
        bank = hbank()
        mm(bank[:], U2m[:], gbuf[:], True, True)
        act(hwk[:, 512:1024], bank[:], AF.Exp)
        tt(kd[:], kbuf[:], hwk[:, 512:1024], ALU.mult)
        yield
        cp(vb[:], pj[:, OIR:OIR + 512])
        vsrc = pj[:, OIR:OIR + 512].rearrange("p (h e) -> p h e", h=4)
        for c in range(4):
            ts(vblk[:, :, c, :], vsrc, cm[:, c:c + 1], None, ALU.mult)
            yield
        bank = hbank()
        for h in range(4):
            mm(bank[:, h * 128:(h + 1) * 128], gbuf[:, h * 128:(h + 1) * 128], Um[:], True, True)
        act(ebT[:], bank[:], AF.Exp)
        act(enbT[:], bank[:], AF.Exp, scale=-1.0)
        yield
        cp(dec[:].rearrange("p (h c) -> p h c", h=4),
           ebT[:].rearrange("p (h c j) -> p h c j", h=4, c=4)[:, :, :, 31], "dve")
        act(hwkb[:, 0:512], pj[:, OQR:OQR + 512], AF.Silu)
        cp(hwkb[:, 512:1024], kbuf[:])
        yield
        bank = hbank(); pb = bank[:].bitcast(BF16)
        for h in range(8):
            tr(pb[:, h * 128:(h + 1) * 128], hwkb[:, h * 128:(h + 1) * 128], identb[:])
        tt(qeT[:].rearrange("p h t -> p (h t)"), pb[:, 0:512], ebT[:], ALU.mult)
        tt(keT[:].rearrange("p h t -> p (h t)"), pb[:, 512:1024], enbT[:], ALU.mult)
        yield
        bank = hbank()
        for h in range(4):
            mm(bank[:, h * 128:(h + 1) * 128], keT[:, h, :], qeT[:, h, :], True, True)
        for h in range(4):
            tt(ATm[:, h, :], bank[:, h * 128:(h + 1) * 128], Um[:], ALU.mult)
            yield
        if (not sample) and first:
            memset(Sst[:, 0, :, :], 0.0)
        for h in range(4):
            bank = hbank()
            mm(bank[:], kd[:, h * 128:(h + 1) * 128], vblk[:, h, :, :].rearrange("p c v -> p (c v)"), True, True)
            for c in range(4):
                sl = Sst[:, (c % 2) if sample else 0, h, :]
                if sample:
                    load(sl, state0[c, h])
                cp(Sb[:, h, c, :], sl, "act")
                stt(sl, sl, dec[:, h * 4 + c:h * 4 + c + 1], bank[:, c * 128:(c + 1) * 128], ALU.mult, ALU.add)
                if sample:
                    store(s_s[c, h], sl)
            yield
        if (not sample) and last:
            store(s_p[seq].rearrange("h k v -> k h v"), Sst[:, 0, :, :])
        obank = hbank()
        for h in range(4):
            mm(obank[:, h * 128:(h + 1) * 128], ATm[:, h, :], vb[:, h * 128:(h + 1) * 128], True, False)
            for c in range(4):
                mm(obank[32 * c:32 * c + 32, h * 128:(h + 1) * 128], qeT[:, h, 32 * c:32 * c + 32], Sb[:, h, c, :],
                   False, True, tile_position=(0, 32 * c))
            yield
        for h in range(4):
            act(ptile[:, 0:128], obank[:, h * 128:(h + 1) * 128], AF.Square, accum_out=st4[:, 16 + h:17 + h])
        act(st4[:, 16:20], st4[:, 16:20], AF.Sqrt, bias=EPS, scale=1.0 / 128)
        recip(st4[:, 16:20], st4[:, 16:20])
        yield
        act(hwk[:, 0:512], pj[:, OGR:OGR + 512], AF.Silu)
        tt(hwk[:, 0:512], hwk[:, 0:512], grec4[:].rearrange("p h e -> p (h e)"), ALU.mult)
        for h in range(4):
            stt(hwkb[:, h * 128:(h + 1) * 128], obank[:, h * 128:(h + 1) * 128], st4[:, 16 + h:17 + h],
                hwk[:, h * 128:(h + 1) * 128], ALU.mult, ALU.mult)
        bank = hbank(); pb = bank[:].bitcast(BF16)
        for h in range(4):
            tr(pb[:, h * 128:(h + 1) * 128], hwkb[:, h * 128:(h + 1) * 128], identb[:])
        cp(yrT2[:, ti, :, :].rearrange("p h t -> p (h t)"), pb[:, 0:512])
        yield

    def stage_merge_tile(ti, Wa, Wr):
        pj = proj[:, ti, :]
        for half in range(2):
            cs = slice(half * 512, (half + 1) * 512)
            ba = gbank()
            for c in range(4):
                mm(ba[:], yaT2[:, ti, c, :], Wa[:, c, cs], c == 0, c == 3)
            br = gbank()
            for c in range(4):
                mm(br[:], yrT2[:, ti, c, :], Wr[:, c, cs], c == 0, c == 3)
            act(wk[:, 0:512], pj[:, OGA + half * 512:OGA + (half + 1) * 512], AF.Sigmoid)
            act(wk[:, 512:1024], pj[:, OGRR + half * 512:OGRR + (half + 1) * 512], AF.Sigmoid)
            tt(wk[:, 0:512], wk[:, 0:512], ba[:], ALU.mult)
            tt(wk[:, 512:1024], wk[:, 512:1024], br[:], ALU.mult)
            tt(merged[:, cs], wk[:, 0:512], wk[:, 512:1024], ALU.add)
        bank = gbank(); pb = bank[:].bitcast(BF16)
        for c in range(8):
            tr(pb[:, c * 128:(c + 1) * 128], merged[:, c * 128:(c + 1) * 128], identb[:])
        cp(xT[:, :, ti * 128:(ti + 1) * 128], pb.rearrange("p (c t) -> p c t", c=8))

    def lin_accum(ntile, wview, src_T):
        wv = wview.rearrange("(c p) n -> p c n", p=128)
        for half in range(2):
            slot = wslot().rearrange("p (c n) -> p c n", c=8)
            load(slot, wv[:, :, half * 512:(half + 1) * 512])
            for ti in range(ntile):
                bank = gbank()
                for c in range(8):
                    mm(bank[:], src_T[:, c, ti * 128:(ti + 1) * 128], slot[:, c, :], c == 0, c == 7)
                tt(xt[:, ti, half * 512:(half + 1) * 512], xt[:, ti, half * 512:(half + 1) * 512], bank[:], ALU.add)

    def stage_route(ti):
        rstd_of(xt[:, ti, :], D, st4[:, 0:1])
        stt(wk[:], xt[:, ti, :], st4[:, 0:1], gbh[:, 1, :], ALU.mult, ALU.mult)
        for half in range(2):
            bank = gbank()
            for c in range(4):
                tr(bank[:, c * 128:(c + 1) * 128], wk[:, (half * 4 + c) * 128:(half * 4 + c + 1) * 128], ident[:])
            cp(hTf[:, half * 4:(half + 1) * 4, :], bank[:].rearrange("p (c t) -> p c t", c=4))
            cp(xT[:, half * 4:(half + 1) * 4, ti * 128:(ti + 1) * 128], bank[:].rearrange("p (c t) -> p c t", c=4))
        bank = gbank()
        for c in range(8):
            mm(bank[:, 0:20], hTf[:, c, :], wrt[:, c, :], c == 0, c == 7)
        lg = rt[:, 0:20]
        tt(lg, bank[:, 0:20], brt[:], ALU.add)
        rmax(rt[:, 20:21], rt[:, 0:4])
        ts(rt[:, 21:22], rt[:, 20:21], -1.0, None, ALU.mult)
        act(rt[:, 24:28], rt[:, 0:4], AF.Exp, bias=rt[:, 21:22], accum_out=rt[:, 22:23])
        recip(rt[:, 23:24], rt[:, 22:23])
        ts(rt[:, 24:28], rt[:, 0:4], rt[:, 20:21], None, ALU.is_ge)
        ts(rt[:, 28:32], rt[:, 24:28], 1e30, -1e30, ALU.mult, ALU.add)
        le = rt[:, 32:48]
        for g in range(4):
            ts(rt[:, 32 + 4 * g:36 + 4 * g], rt[:, 4 + 4 * g:8 + 4 * g], rt[:, 28 + g:29 + g], None, ALU.add)
        rmax(rt[:, 48:49], le)
        m1 = cmb[:, ti, :]
        ts(m1, le, rt[:, 48:49], None, ALU.is_ge)
        stt(wk2[:, 0:16], m1, -1e30, le, ALU.mult, ALU.add)
        rmax(rt[:, 49:50], wk2[:, 0:16])
        ts(wk2[:, 16:32], wk2[:, 0:16], rt[:, 49:50], None, ALU.is_ge)
        tt(rt[:, 50:51], rt[:, 49:50], rt[:, 48:49], ALU.subtract)
        act(rt[:, 50:51], rt[:, 50:51], AF.Exp)
        ts(rt[:, 50:51], rt[:, 50:51], 1.0, None, ALU.add)
        recip(rt[:, 51:52], rt[:, 50:51])
        ts(rt[:, 52:53], rt[:, 51:52], -1.0, 1.0, ALU.mult, ALU.add)
        tt(rt[:, 51:52], rt[:, 51:52], rt[:, 23:24], ALU.mult)
        tt(rt[:, 52:53], rt[:, 52:53], rt[:, 23:24], ALU.mult)
        ts(m1, m1, rt[:, 51:52], None, ALU.mult)
        stt(m1, wk2[:, 16:32], rt[:, 52:53], m1, ALU.mult, ALU.add)

    def stage_experts(ntile):
        ntok = ntile * 128
        bgon = bgs["gen"] is not None
        if bgon:
            cnt["bset"] = 6
        for e in range(16):
            if bgon and e in (0, 2, 5, 8, 11, 14):
                bg_tick()
            s1 = wslot()
            gv = s1[:, 0:2048].rearrange("p (c f) -> p c f", c=8); uv = s1[:, 2048:4096].rearrange("p (c f) -> p c f", c=8)
            load(gv, scr_wg[e].rearrange("(c p) f -> p c f", p=128))
            load(uv, scr_wu[e].rearrange("(c p) f -> p c f", p=128))
            s2 = wslot()
            dv = s2[:, 0:2048].rearrange("p (c n) -> p c n", c=2)
            load(dv, scr_wd[e].rearrange("(c p) n -> p c n", p=128))
            for fc in range(2):
                bank = gbank()
                for c in range(8):
                    mm(bank[:, 0:ntok], gv[:, c, fc * 128:(fc + 1) * 128], xT[:, c, 0:ntok], c == 0, c == 7)
                for c in range(8):
                    mm(bank[:, 256:256 + ntok], uv[:, c, fc * 128:(fc + 1) * 128], xT[:, c, 0:ntok], c == 0, c == 7)
                cnt["sl"] = (cnt["sl"] + 1) % 4
                sgs = wk2[:, cnt["sl"] * 256:cnt["sl"] * 256 + ntok]
                act(sgs, bank[:, 0:ntok], AF.Silu)
                tt(hid[:, e % 2, fc, 0:ntok], sgs, bank[:, 256:256 + ntok], ALU.mult)
            for ti in range(ntile):
                for half in range(2):
                    bank = gbank()
                    for fc in range(2):
                        mm(bank[:], hid[:, e % 2, fc, ti * 128:(ti + 1) * 128], dv[:, fc, half * 512:(half + 1) * 512], fc == 0, fc == 1)
                    xs = xt[:, ti, half * 512:(half + 1) * 512]
                    stt(xs, bank[:], cmb[:, ti, e:e + 1], xs, ALU.mult, ALU.add)
        cnt["bset"] = 8

    def stage_ple_prep(ti, prow, sample):
        rstd_of(xt[:, ti, :], D, st4[:, 0:1])
        stt(wkb[:], xt[:, ti, :], st4[:, 0:1], gbh[:, 2, :], ALU.mult, ALU.mult)
        bank = gbank(); pb = bank[:].bitcast(BF16)
        for c in range(8):
            tr(pb[:, c * 128:(c + 1) * 128], wkb[:, c * 128:(c + 1) * 128], identb[:])
        cp(xT[:, :, ti * 128:(ti + 1) * 128], pb.rearrange("p (c t) -> p c t", c=8))
        if sample:
            for b in range(NSAMP):
                load(ptile[32 * b:32 * b + TS, :], p_s[b * TS:(b + 1) * TS, :])
        else:
            load(ptile[:], p_p[prow:prow + 128, :])
        cp(merged[:, 0:256], ptile[:])
        bank = gbank(); pb = bank[:].bitcast(BF16)
        for c in range(2):
            tr(pb[:, c * 128:(c + 1) * 128], merged[:, c * 128:(c + 1) * 128], identb[:])
        cp(ppT[:, :, ti * 128:(ti + 1) * 128], pb[:, 0:256].rearrange("p (c t) -> p c t", c=2))

    def stage_ple(ntile):
        s3 = wslot()
        wp = s3[:, 0:2048].rearrange("p (c n) -> p c n", c=2)
        load(wp, scr_wple.rearrange("(c p) n -> p c n", p=128))
        wv = scr_wpg.rearrange("(c p) n -> p c n", p=128)
        for half in range(2):
            slot = wslot().rearrange("p (c n) -> p c n", c=8)
            load(slot, wv[:, :, half * 512:(half + 1) * 512])
            for ti in range(ntile):
                b1 = gbank()
                for c in range(8):
                    mm(b1[:], xT[:, c, ti * 128:(ti + 1) * 128], slot[:, c, :], c == 0, c == 7)
                b2 = gbank()
                for c in range(2):
                    mm(b2[:], ppT[:, c, ti * 128:(ti + 1) * 128], wp[:, c, half * 512:(half + 1) * 512], c == 0, c == 1)
                act(wk[:, 0:512], b1[:], AF.Sigmoid)
                tt(wk[:, 0:512], wk[:, 0:512], b2[:], ALU.mult)
                xs = xt[:, ti, half * 512:(half + 1) * 512]
                tt(xs, xs, wk[:, 0:512], ALU.add)

    def stage_final(ti, orow, sample):
        rstd_of(xt[:, ti, :], D, st4[:, 0:1])
        stt(wk[:], xt[:, ti, :], st4[:, 0:1], gfin[:], ALU.mult, ALU.mult)
        if sample:
            for b in range(NSAMP):
                store(y_s[b * TS:(b + 1) * TS, :], wk[32 * b:32 * b + TS, :])
        else:
            store(y_p[orow:orow + 128, :], wk[:])

    kpg = KTt[:].rearrange("p h t -> p (h t)")[:, 0:8192].rearrange("p (b j n) -> p b j n", b=4, j=4)
    vpg = Vpt[:].rearrange("p t h e -> p (t h e)")[:, 0:8192].rearrange("p (b j n) -> p b j n", b=4, j=4)
    _unused = proj[0:64, 1, 0:8]
    kTs = wk
    GP = 8

    projflat = proj[:].rearrange("p a b -> p (a b)")
    kpgB = projflat[:, 0:4096].bitcast(BF16).rearrange("p (b j n) -> p b j n", b=4, j=4)
    vpgB = projflat[:, 4096:8192].bitcast(BF16).rearrange("p (b j n) -> p b j n", b=4, j=4)
    scB = [projflat[0:64, 8192:9224], projflat[0:64, 9224:10256]]
    selB = projflat[0:64, 10256:10768]
    NQ = 2
    NG = NPAGES // 8

    def sgather(dst, src, ix):
        P.dma("pool", dst, src, extra_reads=[ix],
              fn=lambda e: e.indirect_dma_start(out=dst, out_offset=None, in_=src,
                                                in_offset=bass.IndirectOffsetOnAxis(ap=ix, axis=0)))

    def bg_setup(b):
        load(pti, ptab[b].partition_broadcast(128))
        cp(kout[:, 0:NPAGES], pti, "dve")
        ts(kout[:, 0:NPAGES], kout[:, 0:NPAGES], 128.0, posf[:, 0:1], ALU.mult, ALU.add)
        cp(idx[:], kout[:, 0:NPAGES], "dve")
        memset(Zq[:], 0.0)
        for h in range(4):
            for c in range(2):
                cp(Zq[c * 64:(c + 1) * 64, h, h * 16 + c * 8:h * 16 + c * 8 + 8],
                   sqT[c * 64:(c + 1) * 64, h, 32 * b:32 * b + TS], "dve")
        memset(sml[:, 0:1], -1e30); memset(sml[:, 1:2], 0.0); memset(sacc[:], 0.0)

    def bg_gather(u, slot):
        b, g = u
        for q in range(NQ):
            pg0 = g * 8 + q * 4
            for j in range(4):
                sgather(kpgB[:, q + NQ * slot, j, :], cache_k, idx[:, pg0 + j:pg0 + j + 1])
        for q in range(NQ):
            pg0 = g * 8 + q * 4
            for j in range(4):
                sgather(vpgB[:, q + NQ * slot, j, :], cache_v, idx[:, pg0 + j:pg0 + j + 1])

    def bg_softmax(sc, ncols):
        scg = sc[:, 0:ncols]
        m_run = sml[:, 0:1]; l_run = sml[:, 1:2]
        rmax(sml[:, 2:3], scg)
        tt(sml[:, 3:4], sml[:, 2:3], m_run, ALU.max)
        tt(sml[:, 4:5], m_run, sml[:, 3:4], ALU.subtract)
        act(sml[:, 4:5], sml[:, 4:5], AF.Exp, scale=scale)
        ts(sml[:, 5:6], sml[:, 3:4], -scale, None, ALU.mult)
        act(scg, scg, AF.Exp, bias=sml[:, 5:6], scale=scale, accum_out=sml[:, 6:7])
        stt(l_run, l_run, sml[:, 4:5], sml[:, 6:7], ALU.mult, ALU.add)
        cp(m_run, sml[:, 3:4], "dve")

    def bg_acc():
        ts(sacc[:], sacc[:], sml[:, 4:5], None, ALU.mult)
        for h in range(4):
            stt(sacc[:], ps[7][0:64, h * 128:(h + 1) * 128], hm[:, h:h + 1], sacc[:], ALU.mult, ALU.add)

    def bg_kpart(u, slot):
        b, g = u
        sc = scB[slot]
        for q in range(NQ):
            qb = q + NQ * slot
            for h in range(4):
                tb = gbank(); tbb = tb[:].bitcast(BF16)
                for j in range(4):
                    tr(tbb[:, j * 128:(j + 1) * 128], kpgB[:, qb, j, h * 128:(h + 1) * 128], identb[:])
                kts = wkb[:, (h % 2) * 512:(h % 2 + 1) * 512]
                cp(kts, tbb[:, 0:512])
                mm(ps[6][0:64, :], Zq[:, h, :], kts, h == 0, h == 3)
            cp(sc[:, q * 512:(q + 1) * 512], ps[6][0:64, :])
        bg_softmax(sc, NQ * 512)

    def bg_pvpart(u, slot):
        b, g = u
        sc = scB[slot]
        for q in range(NQ):
            tb = gbank()
            for j in range(4):
                tr(tb[:, j * 64:(j + 1) * 64], sc[:, (q * 4 + j) * 128:(q * 4 + j + 1) * 128], ident[0:64, 0:64])
            ptb = pT4[:, q % 2, 0:256]
            cp(ptb, tb[:, 0:256])
            for j in range(4):
                mm(ps[7][0:64, :], ptb[:, j * 64:(j + 1) * 64], vpgB[:, q + NQ * slot, j, :],
                   q == 0 and j == 0, q == NQ - 1 and j == 3)
        bg_acc()

    def bg_final(b):
        sc = scB[0]
        for h in range(4):
            mm(ps[6][0:64, 0:8], Zq[:, h, :], skT[:, h, 32 * b:32 * b + TS], h == 0, h == 3)
        ts(kout[0:64, 256:264], tri8[:], 1e30, -1e30, ALU.mult, ALU.add)
        tt(sc[:, 0:8], ps[6][0:64, 0:8], kout[0:64, 256:264], ALU.add)
        bg_softmax(sc, 8)
        tb = gbank()
        tr(tb[0:8, 0:64], sc[:, 0:8], ident[0:64, 0:64])
        cp(kout[0:8, 384:448], tb[0:8, 0:64])
        load(kbuf[0:8, :], v_s[b * TS:(b + 1) * TS, :])
        mm(ps[7][0:64, :], kout[0:8, 384:448], kbuf[0:8, :], True, True)
        bg_acc()
        recip(sml[:, 7:8], sml[:, 1:2])
        stt(sml[:, 8:9], hm[:, 5:6], lamv[0:64, 1:2], hm[:, 4:5], ALU.mult, ALU.add)
        tt(sml[:, 8:9], sml[:, 8:9], sml[:, 7:8], ALU.mult)
        ts(onorm[:], sacc[:], sml[:, 8:9], None, ALU.mult)
        load(selB, c_sel[b])
        tb = gbank()
        for h in range(4):
            mm(tb[:, h * 128:(h + 1) * 128], selB[:, h * 128:(h + 1) * 128], onorm[:], True, True)
        if b == 0:
            cp(sod[:], tb[:], "dve")
        else:
            tt(sod[:], sod[:], tb[:], ALU.add)

    def bg_generator():
        units = [(b, g) for b in range(NSAMP) for g in range(NG)]
        for w0 in range(0, len(units), 4):
            win = units[w0:w0 + 4]
            n = len(win)
            for step in range(n + 2):
                if step - 2 >= 0:
                    u = win[step - 2]
                    bg_pvpart(u, (step - 2) % 2)
                    if u[1] == NG - 1:
                        bg_final(u[0])
                if step < n:
                    u = win[step]
                    if u[1] == 0:
                        bg_setup(u[0])
                    bg_gather(u, step % 2)
                if 0 <= step - 1 < n:
                    bg_kpart(win[step - 1], (step - 1) % 2)
                yield

    bgs = {"gen": None}

    def bg_tick():
        g = bgs["gen"]
        if g is None:
            return
        try:
            next(g)
        except StopIteration:
            bgs["gen"] = None

    def macro(tiles, sample):
        ntile = len(tiles)
        if sample:
            while bgs["gen"] is not None:
                bg_tick()
        if STOP < 2:
            return
        for ti, tinfo in enumerate(tiles):
            stage_in(ti, tinfo["row"], tinfo["row"], sample)
        stage_proj(ntile)
        if STOP < 3:
            return
        def run(g):
            for _ in g:
                pass

        def hg(ti):
            tinfo = tiles[ti]
            return stage_hgrn(ti, sample, tinfo["tseq"] == 0, tinfo["tseq"] == SEQ // 128 - 1, tinfo["seq"])

        pend = None
        for ti, tinfo in enumerate(tiles):
            cnt["ti"] = ti
            if sample:
                cp(od[:], sod[:], "dve")
                subln_to_yaT()
                run(hg(ti))
                continue
            stage_qkv(ti, tinfo["row"], tinfo["pos"], tinfo["tseq"], sample)
            if pend is None:
                run(stage_attn_prompt(tinfo["tseq"]))
            else:
                cnt["inter"] = True
                ga = stage_attn_prompt(tinfo["tseq"]); gh = pend
                alive = [ga, gh]
                while alive:
                    for g in list(alive):
                        try:
                            next(g)
                        except StopIteration:
                            alive.remove(g)
                cnt["inter"] = False
            pend = hg(ti)
        if pend is not None and not sample:
            run(pend)
        for ti, tinfo in enumerate(tiles):
            if ti == 0:
                Wa = wslot().rearrange("p (c n) -> p c n", c=4)
                load(Wa, scr_wba.rearrange("(c p) n -> p c n", p=128))
                Wr = wslot().rearrange("p (c n) -> p c n", c=4)
                load(Wr, scr_wbr.rearrange("(c p) n -> p c n", p=128))
            stage_merge_tile(ti, Wa, Wr)
        if STOP < 6:
            return
        lin_accum(ntile, scr_wout, xT)
        if STOP < 7:
            return
        for ti in range(ntile):
            stage_route(ti)
        if STOP < 8:
            return
        stage_experts(ntile)
        if STOP < 9:
            return
        for ti, tinfo in enumerate(tiles):
            stage_ple_prep(ti, tinfo["row"], sample)
        stage_ple(ntile)
        for ti, tinfo in enumerate(tiles):
            stage_final(ti, tinfo["row"], sample)

    def prepass():
        stage_in(0, 0, 0, True)
        stage_proj(1, jmax=3)
        stage_qkv(0, 0, SEQ, 0, True)
        cp(sqT[:].rearrange("p h t -> p (h t)"), qT[:].rearrange("p h t -> p (h t)"), "dve")
        cp(skT[:].rearrange("p h t -> p (h t)"), keT[:].rearrange("p h t -> p (h t)"), "dve")
        bgs["gen"] = bg_generator()

    return nc, es, P, macro, dict(Vp=Vp, memset=memset, prepass=prepass)


_CACHE = {}


def _get_program():
    if "nc" in _CACHE:
        return _CACHE["nc"]
    nc, es, P, macro, aux = build_program()
    with es:
        aux["memset"](aux["Vp"], 1.0)
        if DO_SAMPLE:
            aux["prepass"]()
        for seq in range(NSEQ if DO_PROMPT else 0):
            for mt in range(SEQ // 256):
                tiles = []
                for k in range(2):
                    tseq = mt * 2 + k
                    tiles.append(dict(row=seq * SEQ + tseq * 128, pos=tseq * 128, tseq=tseq, seq=seq))
                macro(tiles, False)
        if DO_SAMPLE:
            macro([dict(row=0, pos=SEQ, tseq=0, seq=0)], True)
        P.emit()
    _CACHE["nc"] = nc
    _CACHE["stats"] = P.stats
    return nc


def _consts():
    f = np.float32
    ident = np.eye(128, dtype=f)
    s = np.arange(128)[:, None]; t = np.arange(128)[None, :]
    same = (s // 32) == (t // 32)
    U = (same & (s <= t)).astype(f)
    U2 = (same & (s > t)).astype(f)
    caus1 = (s <= t).astype(f)
    caus = np.concatenate([caus1, caus1], axis=1)
    half = 32
    inv_freq = (np.float32(10000.0) ** (-np.arange(half, dtype=f) / np.float32(half))).astype(f)
    pos = np.concatenate([np.arange(SEQ), np.zeros(128)]).astype(f)
    for b in range(NSAMP):
        for tt_ in range(32):
            pos[SEQ + 32 * b + tt_] = PAST + min(tt_, TS - 1)
    ang = (pos[:, None] * inv_freq[None, :]).astype(f)
    cos = np.tile(np.cos(ang).astype(f), (1, 8)); sin = np.tile(np.sin(ang).astype(f), (1, 8))
    cm = np.zeros((128, 8), f)
    for c in range(4):
        cm[32 * c:32 * c + 32, c] = 1
    for b in range(NSAMP):
        cm[32 * b:32 * b + TS, 4] = 1
    sel = np.zeros((64, 16, 128), f)
    hm = np.zeros((64, 8), f)
    tri8 = np.zeros((64, 8), f)
    for h in range(4):
        for c in range(2):
            for tq in range(TS):
                r = h * 16 + c * 8 + tq
                hm[r, h] = 1
                hm[r, 4 + c] = 1
                tri8[r, :tq + 1] = 1
                for b in range(NSAMP):
                    sel[r, b * 4 + h, 32 * b + tq] = 1
    posi = np.arange(128, dtype=np.float32)[:, None]
    return dict(c_ident=ident, c_U=U, c_U2=U2, c_caus=caus, c_cos=np.ascontiguousarray(cos),
                c_sin=np.ascontiguousarray(sin), c_cm=cm, c_sel=np.ascontiguousarray(sel.reshape(64, 4, 512).transpose(1, 0, 2)), c_hm=hm, c_pos=posi,
                c_tri8=tri8)


def _run(inputs, ncores):
    (x_prompt, x_sample, p_prompt, p_sample, cache_k, cache_v, state_hgrn, page_table,
     g_mix, w_in, lam, g_subln, lb_param, g_rec, w_branch_a, w_branch_r, w_out, g_ffn,
     w_route_group, b_route_group, w_route_expert, b_route_expert, w_exp_gate, w_exp_up,
     w_exp_down, g_ple, w_ple_gate, w_ple, g_final) = inputs
    A = lambda a: np.ascontiguousarray(np.asarray(a))
    nc = _get_program()
    consts = _consts()
    shared = dict(
        cache_k=A(cache_k).reshape(NPHYS * PAGE, 512), cache_v=A(cache_v).reshape(NPHYS * PAGE, 512),
        w_in=A(w_in)[0], w_ba=A(w_branch_a)[0], w_br=A(w_branch_r)[0], w_out=A(w_out)[0],
        w_g=A(w_exp_gate)[0], w_u=A(w_exp_up)[0], w_d=A(w_exp_down)[0], w_pg=A(w_ple_gate)[0], w_ple=A(w_ple)[0],
        w_rt=A(np.concatenate([np.asarray(w_route_group)[0], np.asarray(w_route_expert)[0]], axis=1)),
        b_rt=A(np.concatenate([np.asarray(b_route_group)[0], np.asarray(b_route_expert)[0]])[None, :]),
        gains=A(np.stack([np.asarray(g_mix)[0], np.asarray(g_ffn)[0], np.asarray(g_ple)[0], np.asarray(g_final)])),
        gsmall=A(np.stack([np.asarray(g_subln)[0], np.asarray(g_rec)[0]])),
        lam=A(lam).reshape(1, 256), lbp=A(lb_param), **consts)
    xp = A(x_prompt); xs = A(x_sample); pp = A(p_prompt)[0]; psm = A(p_sample)[0]
    st = A(state_hgrn)[0]; pt = A(page_table).astype(np.int32)
    in_maps = []
    for i in range(ncores):
        m = dict(shared)
        m["x_p"] = xp[2 * i:2 * i + 2].reshape(NSEQ * SEQ, D)
        m["p_p"] = np.ascontiguousarray(pp[2 * i:2 * i + 2]).reshape(NSEQ * SEQ, 256)
        m["x_s"] = xs[4 * i:4 * i + 4].reshape(NSAMP * TS, D)
        m["p_s"] = np.ascontiguousarray(psm[4 * i:4 * i + 4]).reshape(NSAMP * TS, 256)
        m["state0"] = np.ascontiguousarray(st[4 * i:4 * i + 4])
        m["ptab"] = np.ascontiguousarray(pt[4 * i:4 * i + 4])
        in_maps.append(m)
    res = run_bass_kernel_spmd(nc, in_maps, core_ids=list(range(ncores)))
    R = res.results
    cat = lambda k: np.concatenate([np.asarray(r[k]) for r in R], axis=0)
    nb = 2 * ncores; ns = 4 * ncores
    y_prompt = cat("y_p").reshape(nb, SEQ, D)
    y_sample = cat("y_s").reshape(ns, TS, D)
    k_prompt = cat("k_p").reshape(1, nb, SEQ, 4, 2, 64)
    v_prompt = cat("v_p").reshape(1, nb, SEQ, 4, 128)
    s_prompt = cat("s_p").reshape(1, nb, 4, 128, 128)
    k_sample = cat("k_s").reshape(1, ns, TS, 4, 2, 64)
    v_sample = cat("v_s").reshape(1, ns, TS, 4, 128)
    s_sample = cat("s_s").reshape(1, ns, 4, 128, 128)
    return (y_prompt, y_sample, k_prompt, v_prompt, s_prompt, k_sample, v_sample, s_sample)


def kernel(x_prompt, x_sample, p_prompt, p_sample, cache_k, cache_v, state_hgrn, page_table,
           g_mix, w_in, lam, g_subln, lb_param, g_rec, w_branch_a, w_branch_r, w_out, g_ffn,
           w_route_group, b_route_group, w_route_expert, b_route_expert, w_exp_gate, w_exp_up,
           w_exp_down, g_ple, w_ple_gate, w_ple, g_final):
    return _run((x_prompt, x_sample, p_prompt, p_sample, cache_k, cache_v, state_hgrn, page_table,
                 g_mix, w_in, lam, g_subln, lb_param, g_rec, w_branch_a, w_branch_r, w_out, g_ffn,
                 w_route_group, b_route_group, w_route_expert, b_route_expert, w_exp_gate, w_exp_up,
                 w_exp_down, g_ple, w_ple_gate, w_ple, g_final), 8)
```

```python
import contextlib
import os
DBG = set(os.environ.get("K_DBG", "").split(","))
import math
import numpy as np
import concourse.bass as bass
import concourse.mybir as mybir
from concourse.bass_utils import run_bass_kernel_spmd

F32 = mybir.dt.float32
BF16 = mybir.dt.bfloat16
I32 = mybir.dt.int32
AF = mybir.ActivationFunctionType
ALU = mybir.AluOpType

D = 1024; SEQ = 2048; NSEQ = 2; NSAMP = 4; TS = 8; PAST = 16384; PAGE = 128
DIN = 5632; NPHYS = 5120; NPAGES = 128
DO_SAMPLE = True; DO_PROMPT = True; STOP = 99
EPS = 1e-6
LAM_INIT = 0.8 - 0.6 * math.exp(-0.3 * 0)
OQ, OK_, OV, OQR, OFR, OIR, OGR, OGA, OGRR = 0, 512, 1024, 1536, 2048, 2560, 3072, 3584, 4608


def _region(ap):
    t = ap.tensor
    tn = type(t).__name__
    if tn.startswith("DRam"):
        return ("D:" + t.name, 0, 1, 0, 1)
    if tn.startswith("PSum"):
        return ("P:" + t.name, 0, 128, 0, 1)
    dims = list(ap.ap)
    esz = mybir.dt.size(ap.dtype)
    pstride = dims[0][0]
    off = ap.offset
    if pstride == 0:
        p0 = 0; npart = 1; f0 = off
    else:
        p0 = off // pstride; npart = dims[0][1]; f0 = off % pstride
    ext = 1
    for st, cnt in dims[1:]:
        ext += (cnt - 1) * abs(st)
    return ("S:" + t.name, p0, p0 + npart, f0 * esz, (f0 + ext) * esz)


class Op:
    __slots__ = ("eng", "fn", "deps", "inc", "idx", "dma")

    def __init__(self, eng, fn, dma):
        self.eng = eng; self.fn = fn; self.deps = set(); self.inc = False; self.idx = -1; self.dma = dma


class Prog:
    ENGS = ("pe", "dve", "act", "pool", "sp")

    def __init__(self, nc, n_dma_sems=24):
        self.nc = nc; self.ops = []; self.acc = {}; self.n_dma_sems = n_dma_sems

    def _add(self, eng, fn, reads, writes, dma=False):
        op = Op(eng, fn, dma)
        op.idx = len(self.ops)
        self.ops.append(op)
        for ap in reads:
            self._access(op, ap, False)
        for ap in writes:
            self._access(op, ap, True)
        best = {}
        keep = set()
        for d in op.deps:
            p = self.ops[d]
            if p.dma:
                keep.add(d)
            elif best.get(p.eng, -1) < d:
                best[p.eng] = d
        keep.update(best.values())
        op.deps = keep
        return op

    def _access(self, op, ap, is_write):
        key, p0, p1, f0, f1 = _region(ap)
        if key.startswith("D:") and not (key.startswith("D:scr") or key == "D:v_s"):
            return
        lst = self.acc.get(key)
        if lst is None:
            lst = self.acc[key] = []
        keep = []
        psum = key.startswith("P:")
        for rec in lst:
            q0, q1, g0, g1, oi, w = rec
            ov = not (q1 <= p0 or p1 <= q0 or g1 <= f0 or f1 <= g0)
            if ov and (is_write or w or (psum and self.ops[oi].eng != op.eng)) and oi != op.idx:
                op.deps.add(oi)
            if is_write and ov and q0 >= p0 and q1 <= p1 and g0 >= f0 and g1 <= f1:
                continue
            keep.append(rec)
        keep.append((p0, p1, f0, f1, op.idx, is_write))
        self.acc[key] = keep

    def op(self, eng, fn, reads=(), writes=()):
        return self._add(eng, fn, reads, writes, False)

    def dma(self, eng, out, in_, extra_reads=(), fn=None):
        if fn is None:
            fn = lambda e: e.dma_start(out=out, in_=in_)
        return self._add(eng, fn, [in_] + list(extra_reads), [out], True)

    def emit(self):
        nc = self.nc
        ops = self.ops
        engobj = {"pe": nc.tensor, "dve": nc.vector, "act": nc.scalar, "pool": nc.gpsimd, "sp": nc.sync}
        for op in ops:
            for d in op.deps:
                p = ops[d]
                if p.eng == "pe" and op.eng == "pe" and not p.dma:
                    continue
                p.inc = True
        with contextlib.ExitStack() as st:
            esem = {e: st.enter_context(nc.semaphore("sem_" + e)) for e in self.ENGS}
            nds = self.n_dma_sems
            dsem = [st.enter_context(nc.semaphore("dsem%d" % i)) for i in range(2 * nds)]
            ecount = {e: 0 for e in self.ENGS}
            dcount = [0] * (2 * nds)
            waited = {e: {} for e in self.ENGS}
            nd = 0
            ndq = {"sp": 0, "pool": 0, "act": 0}
            tok = {}

            def wait(eng, key, sem, val):
                w = waited[eng]
                if w.get(key, 0) >= val:
                    return
                engobj[eng].wait_ge(sem, val)
                w[key] = val

            for op in ops:
                e = op.eng
                for d in sorted(op.deps):
                    p = ops[d]
                    if p.eng == "pe" and e == "pe" and not p.dma:
                        continue
                    key, sem, val = tok[d]
                    wait(e, key, sem, val)
                if op.dma:
                    si = (ndq[e] % nds) + (nds if e == "pool" else 0)
                    ndq[e] += 1
                    nd += 1
                    if dcount[si]:
                        wait(e, "d%d" % si, dsem[si], dcount[si])
                    ins = op.fn(engobj[e])
                    dcount[si] += 16
                    ins.then_inc(dsem[si], 16)
                    tok[op.idx] = ("d%d" % si, dsem[si], dcount[si])
                else:
                    ins = op.fn(engobj[e])
                    if op.inc:
                        ecount[e] += 1
                        ins.then_inc(esem[e], 1)
                        tok[op.idx] = ("e" + e, esem[e], ecount[e])
            for si in range(2 * nds):
                if dcount[si]:
                    wait("sp", "d%d" % si, dsem[si], dcount[si])
            self.stats = dict(ecount=dict(ecount), ndma=nd, nops=len(ops))


def build_program():
    nc = bass.Bass("TRN2", target_bir_lowering=False)
    es = contextlib.ExitStack()

    def din(name, shape, dt=F32):
        return nc.dram_tensor(name, list(shape), dt, kind="ExternalInput").ap()

    def dout(name, shape, dt=F32):
        return nc.dram_tensor(name, list(shape), dt, kind="ExternalOutput").ap()

    x_p = din("x_p", [NSEQ * SEQ, D]); x_s = din("x_s", [NSAMP * TS, D])
    p_p = din("p_p", [NSEQ * SEQ, 256]); p_s = din("p_s", [NSAMP * TS, 256])
    cache_k = din("cache_k", [NPHYS * PAGE, 512]); cache_v = din("cache_v", [NPHYS * PAGE, 512])
    state0 = din("state0", [NSAMP, 4, 128, 128]); ptab = din("ptab", [NSAMP, NPAGES], I32)
    w_in = din("w_in", [D, DIN]); w_ba = din("w_ba", [512, D]); w_br = din("w_br", [512, D])
    w_out = din("w_out", [D, D]); w_g = din("w_g", [16, D, 256]); w_u = din("w_u", [16, D, 256])
    w_d = din("w_d", [16, 256, D]); w_pg = din("w_pg", [D, D]); w_ple = din("w_ple", [256, D])
    w_rt = din("w_rt", [D, 20]); b_rt = din("b_rt", [1, 20])
    gains = din("gains", [4, D])
    gsmall = din("gsmall", [2, 128])
    lam = din("lam", [1, 256]); lbp = din("lbp", [2, 512])
    c_ident = din("c_ident", [128, 128]); c_U = din("c_U", [128, 128]); c_U2 = din("c_U2", [128, 128])
    c_caus = din("c_caus", [128, 256])
    c_cos = din("c_cos", [SEQ + 128, 256]); c_sin = din("c_sin", [SEQ + 128, 256])
    c_cm = din("c_cm", [128, 8])
    c_sel = din("c_sel", [NSAMP, 64, 512]); c_hm = din("c_hm", [64, 8]); c_pos = din("c_pos", [128, 1])
    c_tri8 = din("c_tri8", [64, 8])

    y_p = dout("y_p", [NSEQ * SEQ, D]); y_s = dout("y_s", [NSAMP * TS, D])
    k_p = dout("k_p", [NSEQ * SEQ, 512]); v_p = dout("v_p", [NSEQ * SEQ, 512])
    s_p = dout("s_p", [NSEQ, 4, 128, 128]); k_s = dout("k_s", [NSAMP * TS, 512])
    v_s = dout("v_s", [NSAMP * TS, 512]); s_s = dout("s_s", [NSAMP, 4, 128, 128])

    def scr(name, shape):
        return nc.dram_tensor("scr_" + name, list(shape), BF16, kind="Internal").ap()

    scr_win = scr("win", [D, DIN]); scr_wba = scr("wba", [512, D]); scr_wbr = scr("wbr", [512, D])
    scr_wout = scr("wout", [D, D]); scr_wg = scr("wg", [16, D, 256]); scr_wu = scr("wu", [16, D, 256])
    scr_wd = scr("wd", [16, 256, D]); scr_wpg = scr("wpg", [D, D]); scr_wple = scr("wple", [256, D])

    def sb(name, shape, dt=F32):
        return es.enter_context(nc.sbuf_tensor(name, list(shape), dt))

    ring = sb("ring", [128, 4, 4096], BF16)
    proj = sb("proj", [128, 2, DIN])
    KTt = sb("KTt", [128, 4, 2048], BF16)
    Vpt = sb("Vpt", [128, 16, 4, 130], BF16)
    xt = sb("xt", [128, 2, D])
    xT = sb("xT", [128, 8, 256], BF16)
    hTf = sb("hTf", [128, 8, 128])
    wk = sb("wk", [128, 1024])
    wk2 = sb("wk2", [128, 1024])
    wkb = sb("wkb", [128, 1024], BF16)
    kout = sb("kout", [128, 512])
    qT = sb("qT", [128, 4, 128], BF16)
    qblk = sb("qblk", [128, 4, 2, 128], BF16)
    pT4 = sb("pT4", [128, 4, 512], BF16)
    od = sb("od", [128, 512])
    yaT2 = sb("yaT2", [128, 2, 4, 128], BF16); yrT2 = sb("yrT2", [128, 2, 4, 128], BF16)
    gbuf = sb("gbuf", [128, 512]); kbuf = sb("kbuf", [128, 512])
    kd = sb("kd", [128, 512], BF16); vb = sb("vb", [128, 512], BF16)
    vblk = sb("vblk", [128, 4, 4, 128], BF16)
    ebT = sb("ebT", [128, 512]); enbT = sb("enbT", [128, 512])
    dec = sb("dec", [128, 16])
    qeT = sb("qeT", [128, 4, 128], BF16); keT = sb("keT", [128, 4, 128], BF16)
    ATm = sb("ATm", [128, 4, 128], BF16)
    Sst = sb("Sst", [128, 2, 4, 128])
    Sb = sb("Sb", [128, 4, 4, 128], BF16)
    merged = sb("merged", [128, 1024], BF16)
    hid = sb("hid", [128, 2, 2, 256], BF16)
    cmb = sb("cmb", [128, 2, 16])
    rt = sb("rt", [128, 64])
    st4 = sb("st4", [128, 32])
    ptile = sb("ptile", [128, 256]); ppT = sb("ppT", [128, 2, 256], BF16)
    cosb = sb("cosb", [128, 256]); sinb = sb("sinb", [128, 256])
    ident = sb("ident", [128, 128]); identb = sb("identb", [128, 128], BF16)
    Um = sb("Um", [128, 128]); U2m = sb("U2m", [128, 128]);
    causb = sb("causb", [128, 256], BF16)
    cm = sb("cm", [128, 8])
    gbh = sb("gbh", [128, 3, D], BF16)
    gfin = sb("gfin", [128, D])
    gsub = sb("gsub", [128, 128]); grec4 = sb("grec4", [128, 4, 128])
    lamv = sb("lamv", [128, 4])
    lb_b = sb("lb_b", [128, 512]); oml_b = sb("oml_b", [128, 512])
    wrt = sb("wrt", [128, 8, 20]); brt = sb("brt", [128, 20])
    hm = sb("hm", [64, 8]); posf = sb("posf", [128, 1])
    tri8 = sb("tri8", [64, 8])
    idx = sb("idx", [128, NPAGES], I32)
    pti = kout[:, 256:256 + NPAGES].bitcast(I32)
    Zq = sb("Zq", [128, 4, 64], BF16)
    sacc = sb("sacc", [64, 128]); sml = sb("sml", [64, 16])
    onorm = sb("onorm", [64, 128])
    sqT = sb("sqT", [128, 4, 128], BF16); skT = sb("skT", [128, 4, 128], BF16)
    sod = sb("sod", [128, 512], BF16)

    ps = [es.enter_context(nc.psum_tensor("ps%d" % i, [128, 512], F32)) for i in range(8)]
    P = Prog(nc)
    cnt = {"bank": 0, "ring": 0, "ev": 0, "bset": 8, "sl": 0, "inter": False, "hb": 0, "ti": 0}

    def hbank():
        if cnt["inter"]:
            cnt["hb"] = (cnt["hb"] + 1) % 3
            return ps[cnt["hb"]]
        return gbank()

    def gbank():
        if cnt["inter"]:
            cnt["bank"] = (cnt["bank"] + 1) % 3
            return ps[3 + cnt["bank"]]
        cnt["bank"] = (cnt["bank"] + 1) % cnt["bset"]
        return ps[cnt["bank"]]

    def aps(*xs):
        return [x for x in xs if x is not None and not isinstance(x, (int, float))]

    def mm(out, lhsT, rhs, start, stop, **kw):
        P.op("pe", lambda e: e.matmul(out, lhsT=lhsT, rhs=rhs, start=start, stop=stop, **kw), [lhsT, rhs], [out])

    def tr(out, in_, idn):
        P.op("pe", lambda e: e.transpose(out=out, in_=in_, identity=idn), [in_, idn], [out])

    def act(out, in_, func, bias=None, scale=None, accum_out=None, eng="act"):
        kw = {}
        if bias is not None: kw["bias"] = bias
        if scale is not None: kw["scale"] = scale
        if accum_out is not None: kw["accum_out"] = accum_out
        P.op("act", lambda e: e.activation(out=out, in_=in_, func=func, **kw),
             aps(in_, bias, scale), aps(out, accum_out))

    def tt(out, in0, in1, op, eng="dve"):
        P.op(eng, lambda e: e.tensor_tensor(out=out, in0=in0, in1=in1, op=op), [in0, in1], [out])

    def ts(out, in0, s1, s2, op0, op1=None, eng="dve"):
        if op1 is None:
            P.op(eng, lambda e: e.tensor_scalar(out=out, in0=in0, scalar1=s1, scalar2=None, op0=op0), aps(in0, s1), [out])
        else:
            P.op(eng, lambda e: e.tensor_scalar(out=out, in0=in0, scalar1=s1, scalar2=s2, op0=op0, op1=op1),
                 aps(in0, s1, s2), [out])

    def stt(out, in0, scalar, in1, op0, op1, eng="dve"):
        P.op(eng, lambda e: e.scalar_tensor_tensor(out=out, in0=in0, scalar=scalar, in1=in1, op0=op0, op1=op1),
             aps(in0, scalar, in1), [out])

    def cp(out, in_, eng=None):
        if eng is None:
            cnt["ev"] += 1
            eng = "act" if cnt["ev"] % 2 else "dve"
        if eng == "act":
            P.op("act", lambda e: e.copy(out=out, in_=in_), [in_], [out])
        else:
            P.op(eng, lambda e: e.tensor_copy(out=out, in_=in_), [in_], [out])

    def recip(out, in_):
        P.op("dve", lambda e: e.reciprocal(out=out, in_=in_), [in_], [out])

    def memset(ap, v, eng="dve"):
        P.op(eng, lambda e: e.memset(ap, v), [], [ap])

    def rmax(out, in_):
        P.op("dve", lambda e: e.reduce_max(out=out, in_=in_, axis=mybir.AxisListType.X), [in_], [out])

    def rsum(out, in_):
        P.op("dve", lambda e: e.reduce_sum(out=out, in_=in_, axis=mybir.AxisListType.X), [in_], [out])

    def load(out, in_, eng="sp"):
        P.dma(eng, out, in_)

    def store(out, in_, eng="pool"):
        P.dma(eng, out, in_)

    def wslot():
        cnt["ring"] = (cnt["ring"] + 1) % 4
        return ring[:, cnt["ring"], :]

    def rstd_of(src, n, dst):
        act(wk2[:, 0:n], src, AF.Square, accum_out=st4[:, 15:16])
        act(st4[:, 14:15], st4[:, 15:16], AF.Sqrt, bias=EPS, scale=1.0 / n)
        recip(dst, st4[:, 14:15])

    load(ident[:], c_ident); load(Um[:], c_U); load(U2m[:], c_U2); load(wk2[:, 0:256], c_caus); load(cm[:], c_cm)
    cp(identb[:], ident[:], "dve"); cp(causb[:], wk2[:, 0:256], "dve")
    for i in range(3):
        load(wk[:], gains[i].partition_broadcast(128))
        cp(gbh[:, i, :], wk[:], "dve")
    load(gfin[:], gains[3].partition_broadcast(128))
    load(gsub[:], gsmall[0].partition_broadcast(128))
    for h in range(4):
        load(grec4[:, h, :], gsmall[1].partition_broadcast(128))
    ts(gsub[:], gsub[:], float((1.0 - LAM_INIT) * math.sqrt(1.0)), None, ALU.mult)
    lamb = wk2[:, 256:512]
    load(lamb, lam[0].partition_broadcast(128))
    lbb = wk2[:, 0:1024].rearrange("p (a b) -> p a b", a=2)
    load(wrt[:], w_rt.rearrange("(c p) n -> p c n", p=128)); load(brt[:], b_rt[0].partition_broadcast(128))
    load(hm[:], c_hm); load(posf[:], c_pos); load(tri8[:], c_tri8)
    tt(wk[:, 0:64], lamb[:, 0:64], lamb[:, 64:128], ALU.mult)
    rsum(lamv[:, 2:3], wk[:, 0:64])
    tt(wk[:, 64:128], lamb[:, 128:192], lamb[:, 192:256], ALU.mult)
    rsum(lamv[:, 3:4], wk[:, 64:128])
    act(lamv[:, 2:4], lamv[:, 2:4], AF.Exp)
    tt(lamv[:, 0:1], lamv[:, 2:3], lamv[:, 3:4], ALU.subtract)
    ts(lamv[:, 0:1], lamv[:, 0:1], float(LAM_INIT), None, ALU.add)
    ts(lamv[:, 1:2], lamv[:, 0:1], -1.0, None, ALU.mult)
    load(lbb[:, 0, :], lbp[0].partition_broadcast(128)); load(lbb[:, 1, :], lbp[1].partition_broadcast(128))
    tt(lb_b[:], lbb[:, 1, :], lbb[:, 0, :], ALU.subtract)
    act(lb_b[:], lb_b[:], AF.Exp)
    ts(lb_b[:], lb_b[:], 1.0, None, ALU.add)
    recip(lb_b[:], lb_b[:])
    ts(oml_b[:], lb_b[:], -1.0, 1.0, ALU.mult, ALU.add)

    def cast_rows(src, dst, rows):
        n = src.shape[0]
        for r0 in range(0, n, rows):
            P.dma("pool", dst[r0:r0 + rows], src[r0:r0 + rows])

    if STOP >= 1:
        cast_rows(w_in, scr_win, 128)
        cast_rows(w_ba, scr_wba, 512); cast_rows(w_br, scr_wbr, 512); cast_rows(w_out, scr_wout, 512)
        for e in range(16):
            P.dma("pool", scr_wg[e], w_g[e]); P.dma("pool", scr_wu[e], w_u[e]); P.dma("pool", scr_wd[e], w_d[e])
        cast_rows(w_pg, scr_wpg, 512); cast_rows(w_ple, scr_wple, 256)

    KT = KTt[:, :, 0:SEQ]
    hwk = hTf[:].rearrange("p c t -> p (c t)")
    hwkb = merged
    memset(qblk[:], 0.0)
    Vp = Vpt[:, 0:SEQ // 128]
    scale = 0.125

    def stage_in(ti, xrows, prow, sample):
        if sample:
            memset(xt[:, ti, :], 0.0)
            memset(ptile[:], 0.0)
            for b in range(NSAMP):
                load(xt[32 * b:32 * b + TS, ti, :], x_s[b * TS:(b + 1) * TS, :])
        else:
            load(xt[:, ti, :], x_p[xrows:xrows + 128, :])
        rstd_of(xt[:, ti, :], D, st4[:, 0:1])
        stt(wkb[:], xt[:, ti, :], st4[:, 0:1], gbh[:, 0, :], ALU.mult, ALU.mult)
        bank = gbank()
        pb = bank[:].bitcast(BF16)
        for c in range(8):
            tr(pb[:, c * 128:(c + 1) * 128], wkb[:, c * 128:(c + 1) * 128], identb[:])
        cp(xT[:, :, ti * 128:(ti + 1) * 128], pb.rearrange("p (c t) -> p c t", c=8))

    def stage_proj(ntile, jmax=11):
        wv = scr_win.rearrange("(c p) n -> p c n", p=128)
        for j in range(jmax):
            slot = wslot().rearrange("p (c n) -> p c n", c=8)
            load(slot, wv[:, :, j * 512:(j + 1) * 512])
            for ti in range(ntile):
                bank = gbank()
                for c in range(8):
                    mm(bank[:], xT[:, c, ti * 128:(ti + 1) * 128], slot[:, c, :], c == 0, c == 7)
                cp(proj[:, ti, j * 512:(j + 1) * 512], bank[:])

    def rope(dst, src, cs, sn):
        s4 = src.rearrange("p (g two d) -> p g two d", g=8, two=2)
        d4 = dst.rearrange("p (g two d) -> p g two d", g=8, two=2)
        c3 = cs.rearrange("p (g d) -> p g d", g=8); s3 = sn.rearrange("p (g d) -> p g d", g=8)
        t1 = wk[:, 0:256].rearrange("p (g d) -> p g d", g=8)
        t2 = wk[:, 256:512].rearrange("p (g d) -> p g d", g=8)
        t3 = wk[:, 512:768].rearrange("p (g d) -> p g d", g=8)
        t4 = wk[:, 768:1024].rearrange("p (g d) -> p g d", g=8)
        tt(t1, s4[:, :, 0, :], c3, ALU.mult)
        tt(t2, s4[:, :, 1, :], s3, ALU.mult)
        tt(t3, s4[:, :, 1, :], c3, ALU.mult)
        tt(t4, s4[:, :, 0, :], s3, ALU.mult)
        tt(d4[:, :, 0, :], t1, t2, ALU.subtract)
        tt(d4[:, :, 1, :], t3, t4, ALU.add)

    def stage_qkv(ti, orow, posrow, tseq, sample):
        load(cosb[:], c_cos[posrow:posrow + 128, :]); load(sinb[:], c_sin[posrow:posrow + 128, :])
        rope(kout[:], proj[:, ti, OK_:OK_ + 512], cosb[:], sinb[:])
        if sample:
            for b in range(NSAMP):
                store(k_s[b * TS:(b + 1) * TS, :], kout[32 * b:32 * b + TS, :])
                store(v_s[b * TS:(b + 1) * TS, :], proj[32 * b:32 * b + TS, ti, OV:OV + 512])
        else:
            store(k_p[orow:orow + 128, :], kout[:])
            store(v_p[orow:orow + 128, :], proj[:, ti, OV:OV + 512])
        rope(wk2[:, 0:512], proj[:, ti, OQ:OQ + 512], cosb[:], sinb[:])
        cp(wkb[:, 0:512], wk2[:, 0:512]); cp(wkb[:, 512:1024], kout[:])
        bank = gbank(); pb = bank[:].bitcast(BF16)
        for h in range(8):
            tr(pb[:, h * 128:(h + 1) * 128], wkb[:, h * 128:(h + 1) * 128], identb[:])
        cp(qT[:].rearrange("p h t -> p (h t)"), pb[:, 0:512], "act")
        if not sample:
            cp(qblk[0:64, :, 0, :], pb[0:64, 0:512].rearrange("p (h t) -> p h t", h=4), "act")
            cp(qblk[64:128, :, 1, :], pb[64:128, 0:512].rearrange("p (h t) -> p h t", h=4), "act")
        if sample:
            cp(keT[:].rearrange("p h t -> p (h t)"), pb[:, 512:1024])
            return
        if "noKT" not in DBG:
            if "ktalt" in DBG:
                for h in range(4):
                    cp(merged[:, h * 128:(h + 1) * 128], pb[:, (4 + h) * 128:(5 + h) * 128], "act")
            elif "kt2d" in DBG:
                for h in range(4):
                    cp(KT[:, h, tseq * 128:(tseq + 1) * 128], pb[:, (4 + h) * 128:(5 + h) * 128], "act")
            elif "ktlow" in DBG:
                cp(KT[:, :, tseq * 128:(tseq + 1) * 128], pb[:, 0:512].rearrange("p (h t) -> p h t", h=4), "act")
            else:
                cp(KT[:, :, tseq * 128:(tseq + 1) * 128], pb[:, 512:1024].rearrange("p (h t) -> p h t", h=4),
                   "dve" if "ktdve" in DBG else ("act" if "ktact" in DBG else None))
        if "noVp" not in DBG:
            cp(Vp[:, tseq, :, 0:128], proj[:, ti, OV:OV + 512].rearrange("p (h e) -> p h e", h=4),
               "dve" if "vpdve" in DBG else "act")

    def attn_finish(odv_src0, odv_src1, h, rec2):
        ts(st4[:, 4:5], rec2[1], lamv[:, 1:2], None, ALU.mult)
        ts(wk[:, 0:128], odv_src0, rec2[0], None, ALU.mult)
        stt(od[:, h * 128:(h + 1) * 128], odv_src1, st4[:, 4:5], wk[:, 0:128], ALU.mult, ALU.add)

    def subln_to_yaT():
        for h in range(4):
            act(wk2[:, 0:128], od[:, h * 128:(h + 1) * 128], AF.Square, accum_out=st4[:, 8 + h:9 + h])
        act(st4[:, 8:12], st4[:, 8:12], AF.Sqrt, bias=EPS, scale=1.0 / 128)
        recip(st4[:, 8:12], st4[:, 8:12])
        for h in range(4):
            stt(wkb[:, h * 128:(h + 1) * 128], od[:, h * 128:(h + 1) * 128], st4[:, 8 + h:9 + h], gsub[:], ALU.mult, ALU.mult)
        bank = gbank(); pb = bank[:].bitcast(BF16)
        for h in range(4):
            tr(pb[:, h * 128:(h + 1) * 128], wkb[:, h * 128:(h + 1) * 128], identb[:])
        cp(yaT2[:, cnt["ti"], :, :].rearrange("p h t -> p (h t)"), pb[:, 0:512])

    def stage_attn_prompt(tseq):
        cnt["bset"] = 2
        nk = tseq + 1
        steps = []
        for h in range(4):
            i = 0
            while i < nk:
                n = 2 if i + 1 < nk else 1
                steps.append((h, i, n)); i += n
        if cnt["inter"]:
            sbanks = [ps[3], ps[4], ps[5]]
            accs = [(ps[6], ps[7]), (ps[6], ps[7])]
        else:
            sbanks = [ps[2], ps[3], ps[4], ps[5]]
            accs = [(ps[6], ps[7]), (ps[0], ps[1])]
        nsb = len(sbanks)
        LA = 2

        def S_E(k):
            h, i0, n = steps[k]
            bank = sbanks[k % nsb]
            for u in range(n):
                mm(bank[:, u * 256:(u + 1) * 256], KT[:, h, (i0 + u) * 128:(i0 + u + 1) * 128],
                   qblk[:, h, :, :].rearrange("p c t -> p (c t)"), True, True)
            pt = pT4[:, k % 4, 0:n * 256]
            act(pt, bank[:, 0:n * 256], AF.Exp, scale=scale)
            if i0 + n - 1 == tseq:
                pd = pT4[:, k % 4, (n - 1) * 256:n * 256]
                tt(pd, pd, causb[:], ALU.mult)

        def PV(k):
            h, i0, n = steps[k]
            a0, a1 = accs[h % 2]
            for u in range(n):
                i = i0 + u
                for c, ab in enumerate((a0, a1)):
                    mm(ab[:, 0:130], pT4[:, k % 4, u * 256 + c * 128:u * 256 + (c + 1) * 128], Vp[:, i, h, :],
                       i == 0, i == tseq)
            if i0 + n - 1 == tseq:
                recip(st4[:, 5:6], a0[:, 128:129]); recip(st4[:, 6:7], a1[:, 128:129])
                attn_finish(a0[:, 0:128], a1[:, 0:128], h, (st4[:, 5:6], st4[:, 6:7]))

        for k in range(len(steps) + LA):
            if k < len(steps):
                S_E(k)
            if k - LA >= 0:
                PV(k - LA)
            yield
        subln_to_yaT()
        cnt["bset"] = 8
        yield

    def stage_hgrn(ti, sample, first, last, seq):
        pj = proj[:, ti, :]
        act(hwk[:, 0:512], pj[:, OFR:OFR + 512], AF.Sigmoid)
        tt(hwk[:, 0:512], hwk[:, 0:512], oml_b[:], ALU.mult)
        tt(hwk[:, 0:512], hwk[:, 0:512], lb_b[:], ALU.add)
        act(gbuf[:], hwk[:, 0:512], AF.Ln)
        ts(kbuf[:], hwk[:, 0:512], -1.0, 1.0, ALU.mult, ALU.add)
        yield
        if sample:
            ts(gbuf[:], gbuf[:], cm[:, 4:5], None, ALU.mult)
            ts(kbuf[:], kbuf[:], cm[:, 4:5], None, ALU.mult)
        bank = hbank()
        mm(bank[:], U2m[:], gbuf[:], True, True)
        act(hwk[:, 512:1024], bank[:], AF.Exp)
        tt(kd[:], kbuf[:], hwk[:, 512:1024], ALU.mult)
        yield
        cp(vb[:], pj[:, OIR:OIR + 512])
        vsrc = pj[:, OIR:OIR + 512].rearrange("p (h e) -> p h e", h=4)
        for c in range(4):
            ts(vblk[:, :, c, :], vsrc, cm[:, c:c + 1], None, ALU.mult)
            yield
        bank = hbank()
        for h in range(4):
            mm(bank[:, h * 128:(h + 1) * 128], gbuf[:, h * 128:(h + 1) * 128], Um[:], True, True)
        act(ebT[:], bank[:], AF.Exp)
        act(enbT[:], bank[:], AF.Exp, scale=-1.0)
        yield
        cp(dec[:].rearrange("p (h c) -> p h c", h=4),
           ebT[:].rearrange("p (h c j) -> p h c j", h=4, c=4)[:, :, :, 31], "dve")
        act(hwkb[:, 0:512], pj[:, OQR:OQR + 512], AF.Silu)
        cp(hwkb[:, 512:1024], kbuf[:])
        yield
        bank = hbank(); pb = bank[:].bitcast(BF16)
        for h in range(8):
            tr(pb[:, h * 128:(h + 1) * 128], hwkb[:, h * 128:(h + 1) * 128], identb[:])
        tt(qeT[:].rearrange("p h t -> p (h t)"), pb[:, 0:512], ebT[:], ALU.mult)
        tt(keT[:].rearrange("p h t -> p (h t)"), pb[:, 512:1024], enbT[:], ALU.mult)
        yield
        bank = hbank()
        for h in range(4):
            mm(bank[:, h * 128:(h + 1) * 128], keT[:, h, :], qeT[:, h, :], True, True)
        for h in range(4):
            tt(ATm[:, h, :], bank[:, h * 128:(h + 1) * 128], Um[:], ALU.mult)
            yield
        if (not sample) and first:
            memset(Sst[:, 0, :, :], 0.0)
        for h in range(4):
            bank = hbank()
            mm(bank[:], kd[:, h * 128:(h + 1) * 128], vblk[:, h, :, :].rearrange("p c v -> p (c v)"), True, True)
            for c in range(4):
                sl = Sst[:, (c % 2) if sample else 0, h, :]
                if sample:
                    load(sl, state0[c, h])
                cp(Sb[:, h, c, :], sl, "act")
                stt(sl, sl, dec[:, h * 4 + c:h * 4 + c + 1], bank[:, c * 128:(c + 1) * 128], ALU.mult, ALU.add)
                if sample:
                    store(s_s[c, h], sl)
            yield
        if (not sample) and last:
            store(s_p[seq].rearrange("h k v -> k h v"), Sst[:, 0, :, :])
        obank = hbank()
        for h in range(4):
            mm(obank[:, h * 128:(h + 1) * 128], ATm[:, h, :], vb[:, h * 128:(h + 1) * 128], True, False)
            for c in range(4):
                mm(obank[32 * c:32 * c + 32, h * 128:(h + 1) * 128], qeT[:, h, 32 * c:32 * c + 32], Sb[:, h, c, :],
                   False, True, tile_position=(0, 32 * c))
            yield
        for h in range(4):
            act(ptile[:, 0:128], obank[:, h * 128:(h + 1) * 128], AF.Square, accum_out=st4[:, 16 + h:17 + h])
        act(st4[:, 16:20], st4[:, 16:20], AF.Sqrt, bias=EPS, scale=1.0 / 128)
        recip(st4[:, 16:20], st4[:, 16:20])
        yield
        act(hwk[:, 0:512], pj[:, OGR:OGR + 512], AF.Silu)
        tt(hwk[:, 0:512], hwk[:, 0:512], grec4[:].rearrange("p h e -> p (h e)"), ALU.mult)
        for h in range(4):
            stt(hwkb[:, h * 128:(h + 1) * 128], obank[:, h * 128:(h + 1) * 128], st4[:, 16 + h:17 + h],
                hwk[:, h * 128:(h + 1) * 128], ALU.mult, ALU.mult)
        bank = hbank(); pb = bank[:].bitcast(BF16)
        for h in range(4):
            tr(pb[:, h * 128:(h + 1) * 128], hwkb[:, h * 128:(h + 1) * 128], identb[:])
        cp(yrT2[:, ti, :, :].rearrange("p h t -> p (h t)"), pb[:, 0:512])
        yield

    def stage_merge_tile(ti, Wa, Wr):
        pj = proj[:, ti, :]
        for half in range(2):
            cs = slice(half * 512, (half + 1) * 512)
            ba = gbank()
            for c in range(4):
                mm(ba[:], yaT2[:, ti, c, :], Wa[:, c, cs], c == 0, c == 3)
            br = gbank()
            for c in range(4):
                mm(br[:], yrT2[:, ti, c, :], Wr[:, c, cs], c == 0, c == 3)
            act(wk[:, 0:512], pj[:, OGA + half * 512:OGA + (half + 1) * 512], AF.Sigmoid)
            act(wk[:, 512:1024], pj[:, OGRR + half * 512:OGRR + (half + 1) * 512], AF.Sigmoid)
            tt(wk[:, 0:512], wk[:, 0:512], ba[:], ALU.mult)
            tt(wk[:, 512:1024], wk[:, 512:1024], br[:], ALU.mult)
            tt(merged[:, cs], wk[:, 0:512], wk[:, 512:1024], ALU.add)
        bank = gbank(); pb = bank[:].bitcast(BF16)
        for c in range(8):
            tr(pb[:, c * 128:(c + 1) * 128], merged[:, c * 128:(c + 1) * 128], identb[:])
        cp(xT[:, :, ti * 128:(ti + 1) * 128], pb.rearrange("p (c t) -> p c t", c=8))

    def lin_accum(ntile, wview, src_T):
        wv = wview.rearrange("(c p) n -> p c n", p=128)
        for half in range(2):
            slot = wslot().rearrange("p (c n) -> p c n", c=8)
            load(slot, wv[:, :, half * 512:(half + 1) * 512])
            for ti in range(ntile):
                bank = gbank()
                for c in range(8):
                    mm(bank[:], src_T[:, c, ti * 128:(ti + 1) * 128], slot[:, c, :], c == 0, c == 7)
                tt(xt[:, ti, half * 512:(half + 1) * 512], xt[:, ti, half * 512:(half + 1) * 512], bank[:], ALU.add)

    def stage_route(ti):
        rstd_of(xt[:, ti, :], D, st4[:, 0:1])
        stt(wk[:], xt[:, ti, :], st4[:, 0:1], gbh[:, 1, :], ALU.mult, ALU.mult)
        for half in range(2):
            bank = gbank()
            for c in range(4):
                tr(bank[:, c * 128:(c + 1) * 128], wk[:, (half * 4 + c) * 128:(half * 4 + c + 1) * 128], ident[:])
            cp(hTf[:, half * 4:(half + 1) * 4, :], bank[:].rearrange("p (c t) -> p c t", c=4))
            cp(xT[:, half * 4:(half + 1) * 4, ti * 128:(ti + 1) * 128], bank[:].rearrange("p (c t) -> p c t", c=4))
        bank = gbank()
        for c in range(8):
            mm(bank[:, 0:20], hTf[:, c, :], wrt[:, c, :], c == 0, c == 7)
        lg = rt[:, 0:20]
        tt(lg, bank[:, 0:20], brt[:], ALU.add)
        rmax(rt[:, 20:21], rt[:, 0:4])
        ts(rt[:, 21:22], rt[:, 20:21], -1.0, None, ALU.mult)
        act(rt[:, 24:28], rt[:, 0:4], AF.Exp, bias=rt[:, 21:22], accum_out=rt[:, 22:23])
        recip(rt[:, 23:24], rt[:, 22:23])
        ts(rt[:, 24:28], rt[:, 0:4], rt[:, 20:21], None, ALU.is_ge)
        ts(rt[:, 28:32], rt[:, 24:28], 1e30, -1e30, ALU.mult, ALU.add)
        le = rt[:, 32:48]
        for g in range(4):
            ts(rt[:, 32 + 4 * g:36 + 4 * g], rt[:, 4 + 4 * g:8 + 4 * g], rt[:, 28 + g:29 + g], None, ALU.add)
        rmax(rt[:, 48:49], le)
        m1 = cmb[:, ti, :]
        ts(m1, le, rt[:, 48:49], None, ALU.is_ge)
        stt(wk2[:, 0:16], m1, -1e30, le, ALU.mult, ALU.add)
        rmax(rt[:, 49:50], wk2[:, 0:16])
        ts(wk2[:, 16:32], wk2[:, 0:16], rt[:, 49:50], None, ALU.is_ge)
        tt(rt[:, 50:51], rt[:, 49:50], rt[:, 48:49], ALU.subtract)
        act(rt[:, 50:51], rt[:, 50:51], AF.Exp)
        ts(rt[:, 50:51], rt[:, 50:51], 1.0, None, ALU.add)
        recip(rt[:, 51:52], rt[:, 50:51])
        ts(rt[:, 52:53], rt[:, 51:52], -1.0, 1.0, ALU.mult, ALU.add)
        tt(rt[:, 51:52], rt[:, 51:52], rt[:, 23:24], ALU.mult)
        tt(rt[:, 52:53], rt[:, 52:53], rt[:, 23:24], ALU.mult)
        ts(m1, m1, rt[:, 51:52], None, ALU.mult)
        stt(m1, wk2[:, 16:32], rt[:, 52:53], m1, ALU.mult, ALU.add)

    def stage_experts(ntile):
        ntok = ntile * 128
        bgon = bgs["gen"] is not None
        if bgon:
            cnt["bset"] = 6
        bgs["wdone"] = False
        for e in range(16):
            if bgon:
                bg_tick()
            s1 = wslot()
            gv = s1[:, 0:2048].rearrange("p (c f) -> p c f", c=8); uv = s1[:, 2048:4096].rearrange("p (c f) -> p c f", c=8)
            load(gv, scr_wg[e].rearrange("(c p) f -> p c f", p=128))
            load(uv, scr_wu[e].rearrange("(c p) f -> p c f", p=128))
            s2 = wslot()
            dv = s2[:, 0:2048].rearrange("p (c n) -> p c n", c=2)
            load(dv, scr_wd[e].rearrange("(c p) n -> p c n", p=128))
            for fc in range(2):
                bank = gbank()
                for c in range(8):
                    mm(bank[:, 0:ntok], gv[:, c, fc * 128:(fc + 1) * 128], xT[:, c, 0:ntok], c == 0, c == 7)
                for c in range(8):
                    mm(bank[:, 256:256 + ntok], uv[:, c, fc * 128:(fc + 1) * 128], xT[:, c, 0:ntok], c == 0, c == 7)
                cnt["sl"] = (cnt["sl"] + 1) % 4
                sgs = wk2[:, cnt["sl"] * 256:cnt["sl"] * 256 + ntok]
                act(sgs, bank[:, 0:ntok], AF.Silu)
                tt(hid[:, e % 2, fc, 0:ntok], sgs, bank[:, 256:256 + ntok], ALU.mult)
                if bgon:
                    bg_tick()
            for ti in range(ntile):
                for half in range(2):
                    bank = gbank()
                    for fc in range(2):
                        mm(bank[:], hid[:, e % 2, fc, ti * 128:(ti + 1) * 128], dv[:, fc, half * 512:(half + 1) * 512], fc == 0, fc == 1)
                    xs = xt[:, ti, half * 512:(half + 1) * 512]
                    stt(xs, bank[:], cmb[:, ti, e:e + 1], xs, ALU.mult, ALU.add)
                    if bgon:
                        bg_tick()
        if bgon:
            bg_tick(drain=True)
        cnt["bset"] = 8

    def stage_ple_prep(ti, prow, sample):
        rstd_of(xt[:, ti, :], D, st4[:, 0:1])
        stt(wkb[:], xt[:, ti, :], st4[:, 0:1], gbh[:, 2, :], ALU.mult, ALU.mult)
        bank = gbank(); pb = bank[:].bitcast(BF16)
        for c in range(8):
            tr(pb[:, c * 128:(c + 1) * 128], wkb[:, c * 128:(c + 1) * 128], identb[:])
        cp(xT[:, :, ti * 128:(ti + 1) * 128], pb.rearrange("p (c t) -> p c t", c=8))
        if sample:
            for b in range(NSAMP):
                load(ptile[32 * b:32 * b + TS, :], p_s[b * TS:(b + 1) * TS, :])
        else:
            load(ptile[:], p_p[prow:prow + 128, :])
        cp(merged[:, 0:256], ptile[:])
        bank = gbank(); pb = bank[:].bitcast(BF16)
        for c in range(2):
            tr(pb[:, c * 128:(c + 1) * 128], merged[:, c * 128:(c + 1) * 128], identb[:])
        cp(ppT[:, :, ti * 128:(ti + 1) * 128], pb[:, 0:256].rearrange("p (c t) -> p c t", c=2))

    def stage_ple(ntile):
        s3 = wslot()
        wp = s3[:, 0:2048].rearrange("p (c n) -> p c n", c=2)
        load(wp, scr_wple.rearrange("(c p) n -> p c n", p=128))
        wv = scr_wpg.rearrange("(c p) n -> p c n", p=128)
        for half in range(2):
            slot = wslot().rearrange("p (c n) -> p c n", c=8)
            load(slot, wv[:, :, half * 512:(half + 1) * 512])
            for ti in range(ntile):
                b1 = gbank()
                for c in range(8):
                    mm(b1[:], xT[:, c, ti * 128:(ti + 1) * 128], slot[:, c, :], c == 0, c == 7)
                b2 = gbank()
                for c in range(2):
                    mm(b2[:], ppT[:, c, ti * 128:(ti + 1) * 128], wp[:, c, half * 512:(half + 1) * 512], c == 0, c == 1)
                act(wk[:, 0:512], b1[:], AF.Sigmoid)
                tt(wk[:, 0:512], wk[:, 0:512], b2[:], ALU.mult)
                xs = xt[:, ti, half * 512:(half + 1) * 512]
                tt(xs, xs, wk[:, 0:512], ALU.add)

    def stage_final(ti, orow, sample):
        rstd_of(xt[:, ti, :], D, st4[:, 0:1])
        stt(wk[:], xt[:, ti, :], st4[:, 0:1], gfin[:], ALU.mult, ALU.mult)
        if sample:
            for b in range(NSAMP):
                store(y_s[b * TS:(b + 1) * TS, :], wk[32 * b:32 * b + TS, :])
        else:
            store(y_p[orow:orow + 128, :], wk[:])

    kpg = KTt[:].rearrange("p h t -> p (h t)")[:, 0:8192].rearrange("p (b j n) -> p b j n", b=4, j=4)
    vpg = Vpt[:].rearrange("p t h e -> p (t h e)")[:, 0:8192].rearrange("p (b j n) -> p b j n", b=4, j=4)
    _unused = proj[0:64, 1, 0:8]
    kTs = wk
    GP = 8

    projflat = proj[:].rearrange("p a b -> p (a b)")
    kpgB = projflat[:, 0:4096].bitcast(BF16).rearrange("p (b j n) -> p b j n", b=4, j=4)
    vpgB = projflat[:, 4096:8192].bitcast(BF16).rearrange("p (b j n) -> p b j n", b=4, j=4)
    scB = [projflat[0:64, 8192:9224], projflat[0:64, 9224:10256]]
    selB = projflat[0:64, 10256:10768]
    NQ = 2
    NG = NPAGES // 8

    def sgather(dst, src, ix):
        P.dma("pool", dst, src, extra_reads=[ix],
              fn=lambda e: e.indirect_dma_start(out=dst, out_offset=None, in_=src,
                                                in_offset=bass.IndirectOffsetOnAxis(ap=ix, axis=0)))

    def bg_setup(b):
        load(pti, ptab[b].partition_broadcast(128))
        cp(kout[:, 0:NPAGES], pti, "dve")
        ts(kout[:, 0:NPAGES], kout[:, 0:NPAGES], 128.0, posf[:, 0:1], ALU.mult, ALU.add)
        cp(idx[:], kout[:, 0:NPAGES], "dve")
        memset(Zq[:], 0.0)
        for h in range(4):
            for c in range(2):
                cp(Zq[c * 64:(c + 1) * 64, h, h * 16 + c * 8:h * 16 + c * 8 + 8],
                   sqT[c * 64:(c + 1) * 64, h, 32 * b:32 * b + TS], "dve")
        memset(sml[:, 0:1], -1e30); memset(sml[:, 1:2], 0.0); memset(sacc[:], 0.0)

    def bg_gather(u, slot):
        b, g = u
        for q in range(NQ):
            pg0 = g * 8 + q * 4
            for j in range(4):
                sgather(kpgB[:, q + NQ * slot, j, :], cache_k, idx[:, pg0 + j:pg0 + j + 1])
            yield
        for q in range(NQ):
            pg0 = g * 8 + q * 4
            for j in range(4):
                sgather(vpgB[:, q + NQ * slot, j, :], cache_v, idx[:, pg0 + j:pg0 + j + 1])
            yield

    def bg_softmax(sc, ncols):
        scg = sc[:, 0:ncols]
        m_run = sml[:, 0:1]; l_run = sml[:, 1:2]
        rmax(sml[:, 2:3], scg)
        yield
        tt(sml[:, 3:4], sml[:, 2:3], m_run, ALU.max)
        yield
        tt(sml[:, 4:5], m_run, sml[:, 3:4], ALU.subtract)
        ts(sml[:, 5:6], sml[:, 3:4], -scale, None, ALU.mult)
        yield
        act(sml[:, 4:5], sml[:, 4:5], AF.Exp, scale=scale)
        act(scg, scg, AF.Exp, bias=sml[:, 5:6], scale=scale, accum_out=sml[:, 6:7])
        yield
        stt(l_run, l_run, sml[:, 4:5], sml[:, 6:7], ALU.mult, ALU.add)
        cp(m_run, sml[:, 3:4], "dve")
        yield

    def bg_acc():
        ts(sacc[:], sacc[:], sml[:, 4:5], None, ALU.mult)
        yield
        for h in range(4):
            stt(sacc[:], ps[7][0:64, h * 128:(h + 1) * 128], hm[:, h:h + 1], sacc[:], ALU.mult, ALU.add)
            if h % 2 == 1:
                yield

    def bg_kpart(u, slot):
        b, g = u
        sc = scB[slot]
        for q in range(NQ):
            qb = q + NQ * slot
            for h in range(4):
                tb = gbank(); tbb = tb[:].bitcast(BF16)
                for j in range(4):
                    tr(tbb[:, j * 128:(j + 1) * 128], kpgB[:, qb, j, h * 128:(h + 1) * 128], identb[:])
                kts = wkb[:, (h % 2) * 512:(h % 2 + 1) * 512]
                yield
                cp(kts, tbb[:, 0:512])
                yield
                mm(ps[6][0:64, :], Zq[:, h, :], kts, h == 0, h == 3)
            yield
            cp(sc[:, q * 512:(q + 1) * 512], ps[6][0:64, :])
        yield
        yield from bg_softmax(sc, NQ * 512)

    def bg_pvpart(u, slot):
        b, g = u
        sc = scB[slot]
        for q in range(NQ):
            tb = gbank()
            for j in range(4):
                tr(tb[:, j * 64:(j + 1) * 64], sc[:, (q * 4 + j) * 128:(q * 4 + j + 1) * 128], ident[0:64, 0:64])
            ptb = pT4[:, q % 2, 0:256]
            yield
            cp(ptb, tb[:, 0:256])
            yield
            for j in range(4):
                mm(ps[7][0:64, :], ptb[:, j * 64:(j + 1) * 64], vpgB[:, q + NQ * slot, j, :],
                   q == 0 and j == 0, q == NQ - 1 and j == 3)
            yield
        yield from bg_acc()

    def bg_final(b):
        sc = scB[0]
        for h in range(4):
            mm(ps[6][0:64, 0:8], Zq[:, h, :], skT[:, h, 32 * b:32 * b + TS], h == 0, h == 3)
        ts(kout[0:64, 256:264], tri8[:], 1e30, -1e30, ALU.mult, ALU.add)
        tt(sc[:, 0:8], ps[6][0:64, 0:8], kout[0:64, 256:264], ALU.add)
        yield from bg_softmax(sc, 8)
        tb = gbank()
        tr(tb[0:8, 0:64], sc[:, 0:8], ident[0:64, 0:64])
        cp(kout[0:8, 384:448], tb[0:8, 0:64])
        load(kbuf[0:8, :], v_s[b * TS:(b + 1) * TS, :])
        yield
        mm(ps[7][0:64, :], kout[0:8, 384:448], kbuf[0:8, :], True, True)
        yield from bg_acc()
        recip(sml[:, 7:8], sml[:, 1:2])
        stt(sml[:, 8:9], hm[:, 5:6], lamv[0:64, 1:2], hm[:, 4:5], ALU.mult, ALU.add)
        tt(sml[:, 8:9], sml[:, 8:9], sml[:, 7:8], ALU.mult)
        ts(onorm[:], sacc[:], sml[:, 8:9], None, ALU.mult)
        load(selB, c_sel[b])
        tb = gbank()
        for h in range(4):
            mm(tb[:, h * 128:(h + 1) * 128], selB[:, h * 128:(h + 1) * 128], onorm[:], True, True)
        if b == 0:
            cp(sod[:], tb[:], "dve")
        else:
            tt(sod[:], sod[:], tb[:], ALU.add)
        yield

    def bg_generator():
        units = [(b, g) for b in range(NSAMP) for g in range(NG)]
        for w0 in range(0, len(units), 4):
            win = units[w0:w0 + 4]
            n = len(win)
            for step in range(n + 2):
                if step - 2 >= 0:
                    u = win[step - 2]
                    yield from bg_pvpart(u, (step - 2) % 2)
                    if u[1] == NG - 1:
                        yield from bg_final(u[0])
                if step < n:
                    u = win[step]
                    if u[1] == 0:
                        bg_setup(u[0])
                        yield
                    yield from bg_gather(u, step % 2)
                if 0 <= step - 1 < n:
                    yield from bg_kpart(win[step - 1], (step - 1) % 2)
            yield "WINDOW_END"

    bgs = {"gen": None, "wdone": False}

    def bg_tick(drain=False):
        g = bgs["gen"]
        if g is None or bgs["wdone"]:
            return
        try:
            k = 0
            while True:
                r = next(g)
                k += 1
                if r == "WINDOW_END":
                    bgs["wdone"] = True
                    return
                if not drain and k >= 2:
                    return
        except StopIteration:
            bgs["gen"] = None

    def macro(tiles, sample):
        ntile = len(tiles)
        if sample:
            while bgs["gen"] is not None:
                bgs["wdone"] = False
                bg_tick(drain=True)
        if STOP < 2:
            return
        for ti, tinfo in enumerate(tiles):
            stage_in(ti, tinfo["row"], tinfo["row"], sample)
        stage_proj(ntile)
        if STOP < 3:
            return
        def run(g):
            for _ in g:
                pass

        def hg(ti):
            tinfo = tiles[ti]
            return stage_hgrn(ti, sample, tinfo["tseq"] == 0, tinfo["tseq"] == SEQ // 128 - 1, tinfo["seq"])

        pend = None
        for ti, tinfo in enumerate(tiles):
            cnt["ti"] = ti
            if sample:
                cp(od[:], sod[:], "dve")
                subln_to_yaT()
                run(hg(ti))
                continue
            stage_qkv(ti, tinfo["row"], tinfo["pos"], tinfo["tseq"], sample)
            if pend is None:
                run(stage_attn_prompt(tinfo["tseq"]))
            else:
                cnt["inter"] = True
                ga = stage_attn_prompt(tinfo["tseq"]); gh = pend
                alive = [ga, gh]
                while alive:
                    for g in list(alive):
                        try:
                            next(g)
                        except StopIteration:
                            alive.remove(g)
                cnt["inter"] = False
            pend = hg(ti)
        if pend is not None and not sample:
            run(pend)
        for ti, tinfo in enumerate(tiles):
            if ti == 0:
                Wa = wslot().rearrange("p (c n) -> p c n", c=4)
                load(Wa, scr_wba.rearrange("(c p) n -> p c n", p=128))
                Wr = wslot().rearrange("p (c n) -> p c n", c=4)
                load(Wr, scr_wbr.rearrange("(c p) n -> p c n", p=128))
            stage_merge_tile(ti, Wa, Wr)
        if STOP < 6:
            return
        lin_accum(ntile, scr_wout, xT)
        if STOP < 7:
            return
        for ti in range(ntile):
            stage_route(ti)
        if STOP < 8:
            return
        stage_experts(ntile)
        if STOP < 9:
            return
        for ti, tinfo in enumerate(tiles):
            stage_ple_prep(ti, tinfo["row"], sample)
        stage_ple(ntile)
        for ti, tinfo in enumerate(tiles):
            stage_final(ti, tinfo["row"], sample)

    def prepass():
        stage_in(0, 0, 0, True)
        stage_proj(1, jmax=3)
        stage_qkv(0, 0, SEQ, 0, True)
        cp(sqT[:].rearrange("p h t -> p (h t)"), qT[:].rearrange("p h t -> p (h t)"), "dve")
        cp(skT[:].rearrange("p h t -> p (h t)"), keT[:].rearrange("p h t -> p (h t)"), "dve")
        bgs["gen"] = bg_generator()

    return nc, es, P, macro, dict(Vp=Vp, memset=memset, prepass=prepass)


_CACHE = {}


def _get_program():
    if "nc" in _CACHE:
        return _CACHE["nc"]
    nc, es, P, macro, aux = build_program()
    with es:
        aux["memset"](aux["Vp"], 1.0)
        if DO_SAMPLE:
            aux["prepass"]()
        for seq in range(NSEQ if DO_PROMPT else 0):
            for mt in range(SEQ // 256):
                tiles = []
                for k in range(2):
                    tseq = mt * 2 + k
                    tiles.append(dict(row=seq * SEQ + tseq * 128, pos=tseq * 128, tseq=tseq, seq=seq))
                macro(tiles, False)
        if DO_SAMPLE:
            macro([dict(row=0, pos=SEQ, tseq=0, seq=0)], True)
        P.emit()
    _CACHE["nc"] = nc
    _CACHE["stats"] = P.stats
    return nc


def _consts():
    f = np.float32
    ident = np.eye(128, dtype=f)
    s = np.arange(128)[:, None]; t = np.arange(128)[None, :]
    same = (s // 32) == (t // 32)
    U = (same & (s <= t)).astype(f)
    U2 = (same & (s > t)).astype(f)
    caus1 = (s <= t).astype(f)
    caus = np.concatenate([caus1, caus1], axis=1)
    half = 32
    inv_freq = (np.float32(10000.0) ** (-np.arange(half, dtype=f) / np.float32(half))).astype(f)
    pos = np.concatenate([np.arange(SEQ), np.zeros(128)]).astype(f)
    for b in range(NSAMP):
        for tt_ in range(32):
            pos[SEQ + 32 * b + tt_] = PAST + min(tt_, TS - 1)
    ang = (pos[:, None] * inv_freq[None, :]).astype(f)
    cos = np.tile(np.cos(ang).astype(f), (1, 8)); sin = np.tile(np.sin(ang).astype(f), (1, 8))
    cm = np.zeros((128, 8), f)
    for c in range(4):
        cm[32 * c:32 * c + 32, c] = 1
    for b in range(NSAMP):
        cm[32 * b:32 * b + TS, 4] = 1
    sel = np.zeros((64, 16, 128), f)
    hm = np.zeros((64, 8), f)
    tri8 = np.zeros((64, 8), f)
    for h in range(4):
        for c in range(2):
            for tq in range(TS):
                r = h * 16 + c * 8 + tq
                hm[r, h] = 1
                hm[r, 4 + c] = 1
                tri8[r, :tq + 1] = 1
                for b in range(NSAMP):
                    sel[r, b * 4 + h, 32 * b + tq] = 1
    posi = np.arange(128, dtype=np.float32)[:, None]
    return dict(c_ident=ident, c_U=U, c_U2=U2, c_caus=caus, c_cos=np.ascontiguousarray(cos),
                c_sin=np.ascontiguousarray(sin), c_cm=cm, c_sel=np.ascontiguousarray(sel.reshape(64, 4, 512).transpose(1, 0, 2)), c_hm=hm, c_pos=posi,
                c_tri8=tri8)


def _run(inputs, ncores):
    (x_prompt, x_sample, p_prompt, p_sample, cache_k, cache_v, state_hgrn, page_table,
     g_mix, w_in, lam, g_subln, lb_param, g_rec, w_branch_a, w_branch_r, w_out, g_ffn,
     w_route_group, b_route_group, w_route_expert, b_route_expert, w_exp_gate, w_exp_up,
     w_exp_down, g_ple, w_ple_gate, w_ple, g_final) = inputs
    A = lambda a: np.ascontiguousarray(np.asarray(a))
    nc = _get_program()
    consts = _consts()
    shared = dict(
        cache_k=A(cache_k).reshape(NPHYS * PAGE, 512), cache_v=A(cache_v).reshape(NPHYS * PAGE, 512),
        w_in=A(w_in)[0], w_ba=A(w_branch_a)[0], w_br=A(w_branch_r)[0], w_out=A(w_out)[0],
        w_g=A(w_exp_gate)[0], w_u=A(w_exp_up)[0], w_d=A(w_exp_down)[0], w_pg=A(w_ple_gate)[0], w_ple=A(w_ple)[0],
        w_rt=A(np.concatenate([np.asarray(w_route_group)[0], np.asarray(w_route_expert)[0]], axis=1)),
        b_rt=A(np.concatenate([np.asarray(b_route_group)[0], np.asarray(b_route_expert)[0]])[None, :]),
        gains=A(np.stack([np.asarray(g_mix)[0], np.asarray(g_ffn)[0], np.asarray(g_ple)[0], np.asarray(g_final)])),
        gsmall=A(np.stack([np.asarray(g_subln)[0], np.asarray(g_rec)[0]])),
        lam=A(lam).reshape(1, 256), lbp=A(lb_param), **consts)
    xp = A(x_prompt); xs = A(x_sample); pp = A(p_prompt)[0]; psm = A(p_sample)[0]
    st = A(state_hgrn)[0]; pt = A(page_table).astype(np.int32)
    in_maps = []
    for i in range(ncores):
        m = dict(shared)
        m["x_p"] = xp[2 * i:2 * i + 2].reshape(NSEQ * SEQ, D)
        m["p_p"] = np.ascontiguousarray(pp[2 * i:2 * i + 2]).reshape(NSEQ * SEQ, 256)
        m["x_s"] = xs[4 * i:4 * i + 4].reshape(NSAMP * TS, D)
        m["p_s"] = np.ascontiguousarray(psm[4 * i:4 * i + 4]).reshape(NSAMP * TS, 256)
        m["state0"] = np.ascontiguousarray(st[4 * i:4 * i + 4])
        m["ptab"] = np.ascontiguousarray(pt[4 * i:4 * i + 4])
        in_maps.append(m)
    res = run_bass_kernel_spmd(nc, in_maps, core_ids=list(range(ncores)))
    R = res.results
    cat = lambda k: np.concatenate([np.asarray(r[k]) for r in R], axis=0)
    nb = 2 * ncores; ns = 4 * ncores
    y_prompt = cat("y_p").reshape(nb, SEQ, D)
    y_sample = cat("y_s").reshape(ns, TS, D)
    k_prompt = cat("k_p").reshape(1, nb, SEQ, 4, 2, 64)
    v_prompt = cat("v_p").reshape(1, nb, SEQ, 4, 128)
    s_prompt = cat("s_p").reshape(1, nb, 4, 128, 128)
    k_sample = cat("k_s").reshape(1, ns, TS, 4, 2, 64)
    v_sample = cat("v_s").reshape(1, ns, TS, 4, 128)
    s_sample = cat("s_s").reshape(1, ns, 4, 128, 128)
    return (y_prompt, y_sample, k_prompt, v_prompt, s_prompt, k_sample, v_sample, s_sample)


def kernel(x_prompt, x_sample, p_prompt, p_sample, cache_k, cache_v, state_hgrn, page_table,
           g_mix, w_in, lam, g_subln, lb_param, g_rec, w_branch_a, w_branch_r, w_out, g_ffn,
           w_route_group, b_route_group, w_route_expert, b_route_expert, w_exp_gate, w_exp_up,
           w_exp_down, g_ple, w_ple_gate, w_ple, g_final):
    return _run((x_prompt, x_sample, p_prompt, p_sample, cache_k, cache_v, state_hgrn, page_table,
                 g_mix, w_in, lam, g_subln, lb_param, g_rec, w_branch_a, w_branch_r, w_out, g_ffn,
                 w_route_group, b_route_group, w_route_expert, b_route_expert, w_exp_gate, w_exp_up,
                 w_exp_down, g_ple, w_ple_gate, w_ple, g_final), 8)
```

```python
import contextlib
import os
DBG = set(os.environ.get("K_DBG", "").split(","))
import math
import numpy as np
import concourse.bass as bass
import concourse.mybir as mybir
from concourse.bass_utils import run_bass_kernel_spmd

F32 = mybir.dt.float32
BF16 = mybir.dt.bfloat16
I32 = mybir.dt.int32
AF = mybir.ActivationFunctionType
ALU = mybir.AluOpType

D = 1024; SEQ = 2048; NSEQ = 2; NSAMP = 4; TS = 8; PAST = 16384; PAGE = 128
DIN = 5632; NPHYS = 5120; NPAGES = 128
DO_SAMPLE = True; DO_PROMPT = True; STOP = 99
EPS = 1e-6
LAM_INIT = 0.8 - 0.6 * math.exp(-0.3 * 0)
OQ, OK_, OV, OQR, OFR, OIR, OGR, OGA, OGRR = 0, 512, 1024, 1536, 2048, 2560, 3072, 3584, 4608


def _region(ap):
    t = ap.tensor
    tn = type(t).__name__
    if tn.startswith("DRam"):
        return ("D:" + t.name, 0, 1, 0, 1)
    if tn.startswith("PSum"):
        return ("P:" + t.name, 0, 128, 0, 1)
    dims = list(ap.ap)
    esz = mybir.dt.size(ap.dtype)
    pstride = dims[0][0]
    off = ap.offset
    if pstride == 0:
        p0 = 0; npart = 1; f0 = off
    else:
        p0 = off // pstride; npart = dims[0][1]; f0 = off % pstride
    ext = 1
    for st, cnt in dims[1:]:
        ext += (cnt - 1) * abs(st)
    return ("S:" + t.name, p0, p0 + npart, f0 * esz, (f0 + ext) * esz)


class Op:
    __slots__ = ("eng", "fn", "deps", "inc", "idx", "dma")

    def __init__(self, eng, fn, dma):
        self.eng = eng; self.fn = fn; self.deps = set(); self.inc = False; self.idx = -1; self.dma = dma


class Prog:
    ENGS = ("pe", "dve", "act", "pool", "sp")

    def __init__(self, nc, n_dma_sems=24):
        self.nc = nc; self.ops = []; self.acc = {}; self.n_dma_sems = n_dma_sems

    def _add(self, eng, fn, reads, writes, dma=False):
        op = Op(eng, fn, dma)
        op.idx = len(self.ops)
        self.ops.append(op)
        for ap in reads:
            self._access(op, ap, False)
        for ap in writes:
            self._access(op, ap, True)
        best = {}
        keep = set()
        for d in op.deps:
            p = self.ops[d]
            if p.dma:
                keep.add(d)
            elif best.get(p.eng, -1) < d:
                best[p.eng] = d
        keep.update(best.values())
        op.deps = keep
        return op

    def _access(self, op, ap, is_write):
        key, p0, p1, f0, f1 = _region(ap)
        if key.startswith("D:") and not (key.startswith("D:scr") or key == "D:v_s"):
            return
        lst = self.acc.get(key)
        if lst is None:
            lst = self.acc[key] = []
        keep = []
        psum = key.startswith("P:")
        for rec in lst:
            q0, q1, g0, g1, oi, w = rec
            ov = not (q1 <= p0 or p1 <= q0 or g1 <= f0 or f1 <= g0)
            if ov and (is_write or w or (psum and self.ops[oi].eng != op.eng)) and oi != op.idx:
                op.deps.add(oi)
            if is_write and ov and q0 >= p0 and q1 <= p1 and g0 >= f0 and g1 <= f1:
                continue
            keep.append(rec)
        keep.append((p0, p1, f0, f1, op.idx, is_write))
        self.acc[key] = keep

    def op(self, eng, fn, reads=(), writes=()):
        return self._add(eng, fn, reads, writes, False)

    def dma(self, eng, out, in_, extra_reads=(), fn=None):
        if fn is None:
            fn = lambda e: e.dma_start(out=out, in_=in_)
        return self._add(eng, fn, [in_] + list(extra_reads), [out], True)

    def emit(self):
        nc = self.nc
        ops = self.ops
        engobj = {"pe": nc.tensor, "dve": nc.vector, "act": nc.scalar, "pool": nc.gpsimd, "sp": nc.sync}
        for op in ops:
            for d in op.deps:
                p = ops[d]
                if p.eng == "pe" and op.eng == "pe" and not p.dma:
                    continue
                p.inc = True
        with contextlib.ExitStack() as st:
            esem = {e: st.enter_context(nc.semaphore("sem_" + e)) for e in self.ENGS}
            nds = self.n_dma_sems
            dsem = [st.enter_context(nc.semaphore("dsem%d" % i)) for i in range(2 * nds)]
            ecount = {e: 0 for e in self.ENGS}
            dcount = [0] * (2 * nds)
            waited = {e: {} for e in self.ENGS}
            nd = 0
            ndq = {"sp": 0, "pool": 0, "act": 0}
            tok = {}

            def wait(eng, key, sem, val):
                w = waited[eng]
                if w.get(key, 0) >= val:
                    return
                engobj[eng].wait_ge(sem, val)
                w[key] = val

            for op in ops:
                e = op.eng
                for d in sorted(op.deps):
                    p = ops[d]
                    if p.eng == "pe" and e == "pe" and not p.dma:
                        continue
                    key, sem, val = tok[d]
                    wait(e, key, sem, val)
                if op.dma:
                    si = (ndq[e] % nds) + (nds if e == "pool" else 0)
                    ndq[e] += 1
                    nd += 1
                    if dcount[si]:
                        wait(e, "d%d" % si, dsem[si], dcount[si])
                    ins = op.fn(engobj[e])
                    dcount[si] += 16
                    ins.then_inc(dsem[si], 16)
                    tok[op.idx] = ("d%d" % si, dsem[si], dcount[si])
                else:
                    ins = op.fn(engobj[e])
                    if op.inc:
                        ecount[e] += 1
                        ins.then_inc(esem[e], 1)
                        tok[op.idx] = ("e" + e, esem[e], ecount[e])
            for si in range(2 * nds):
                if dcount[si]:
                    wait("sp", "d%d" % si, dsem[si], dcount[si])
            self.stats = dict(ecount=dict(ecount), ndma=nd, nops=len(ops))


def build_program():
    nc = bass.Bass("TRN2", target_bir_lowering=False)
    es = contextlib.ExitStack()

    def din(name, shape, dt=F32):
        return nc.dram_tensor(name, list(shape), dt, kind="ExternalInput").ap()

    def dout(name, shape, dt=F32):
        return nc.dram_tensor(name, list(shape), dt, kind="ExternalOutput").ap()

    x_p = din("x_p", [NSEQ * SEQ, D]); x_s = din("x_s", [NSAMP * TS, D])
    p_p = din("p_p", [NSEQ * SEQ, 256]); p_s = din("p_s", [NSAMP * TS, 256])
    cache_k = din("cache_k", [NPHYS * PAGE, 512]); cache_v = din("cache_v", [NPHYS * PAGE, 512])
    state0 = din("state0", [NSAMP, 4, 128, 128]); ptab = din("ptab", [NSAMP, NPAGES], I32)
    w_in = din("w_in", [D, DIN]); w_ba = din("w_ba", [512, D]); w_br = din("w_br", [512, D])
    w_out = din("w_out", [D, D]); w_g = din("w_g", [16, D, 256]); w_u = din("w_u", [16, D, 256])
    w_d = din("w_d", [16, 256, D]); w_pg = din("w_pg", [D, D]); w_ple = din("w_ple", [256, D])
    w_rt = din("w_rt", [D, 20]); b_rt = din("b_rt", [1, 20])
    gains = din("gains", [4, D])
    gsmall = din("gsmall", [2, 128])
    lam = din("lam", [1, 256]); lbp = din("lbp", [2, 512])
    c_ident = din("c_ident", [128, 128]); c_U = din("c_U", [128, 128]); c_U2 = din("c_U2", [128, 128])
    c_caus = din("c_caus", [128, 256])
    c_cos = din("c_cos", [SEQ + 128, 256]); c_sin = din("c_sin", [SEQ + 128, 256])
    c_cm = din("c_cm", [128, 8])
    c_sel = din("c_sel", [NSAMP, 64, 512]); c_hm = din("c_hm", [64, 8]); c_pos = din("c_pos", [128, 1])
    c_tri8 = din("c_tri8", [64, 8])

    y_p = dout("y_p", [NSEQ * SEQ, D]); y_s = dout("y_s", [NSAMP * TS, D])
    k_p = dout("k_p", [NSEQ * SEQ, 512]); v_p = dout("v_p", [NSEQ * SEQ, 512])
    s_p = dout("s_p", [NSEQ, 4, 128, 128]); k_s = dout("k_s", [NSAMP * TS, 512])
    v_s = dout("v_s", [NSAMP * TS, 512]); s_s = dout("s_s", [NSAMP, 4, 128, 128])

    def scr(name, shape):
        return nc.dram_tensor("scr_" + name, list(shape), BF16, kind="Internal").ap()

    scr_win = scr("win", [D, DIN]); scr_wba = scr("wba", [512, D]); scr_wbr = scr("wbr", [512, D])
    scr_wout = scr("wout", [D, D]); scr_wg = scr("wg", [16, D, 256]); scr_wu = scr("wu", [16, D, 256])
    scr_wd = scr("wd", [16, 256, D]); scr_wpg = scr("wpg", [D, D]); scr_wple = scr("wple", [256, D])

    def sb(name, shape, dt=F32):
        return es.enter_context(nc.sbuf_tensor(name, list(shape), dt))

    ring = sb("ring", [128, 4, 4096], BF16)
    proj = sb("proj", [128, 2, DIN])
    KTt = sb("KTt", [128, 4, 2048], BF16)
    Vpt = sb("Vpt", [128, 16, 4, 130], BF16)
    xt = sb("xt", [128, 2, D])
    xT = sb("xT", [128, 8, 256], BF16)
    hTf = sb("hTf", [128, 8, 128])
    wk = sb("wk", [128, 1024])
    wk2 = sb("wk2", [128, 1024])
    wkb = sb("wkb", [128, 1024], BF16)
    kout = sb("kout", [128, 512])
    qT = sb("qT", [128, 4, 128], BF16)
    qblk = sb("qblk", [128, 4, 2, 128], BF16)
    pT4 = sb("pT4", [128, 4, 512], BF16)
    od = sb("od", [128, 512])
    yaT2 = sb("yaT2", [128, 2, 4, 128], BF16); yrT2 = sb("yrT2", [128, 2, 4, 128], BF16)
    gbuf = sb("gbuf", [128, 512]); kbuf = sb("kbuf", [128, 512])
    kd = sb("kd", [128, 512], BF16); vb = sb("vb", [128, 512], BF16)
    vblk = sb("vblk", [128, 4, 4, 128], BF16)
    ebT = sb("ebT", [128, 512]); enbT = sb("enbT", [128, 512])
    dec = sb("dec", [128, 16])
    qeT = sb("qeT", [128, 4, 128], BF16); keT = sb("keT", [128, 4, 128], BF16)
    ATm = sb("ATm", [128, 4, 128], BF16)
    Sst = sb("Sst", [128, 2, 4, 128])
    Sb = sb("Sb", [128, 4, 4, 128], BF16)
    merged = sb("merged", [128, 1024], BF16)
    hid = sb("hid", [128, 2, 2, 256], BF16)
    cmb = sb("cmb", [128, 2, 16])
    rt = sb("rt", [128, 64])
    st4 = sb("st4", [128, 32])
    ptile = sb("ptile", [128, 256]); ppT = sb("ppT", [128, 2, 256], BF16)
    cosb = sb("cosb", [128, 256]); sinb = sb("sinb", [128, 256])
    ident = sb("ident", [128, 128]); identb = sb("identb", [128, 128], BF16)
    Um = sb("Um", [128, 128]); U2m = sb("U2m", [128, 128]);
    causb = sb("causb", [128, 256], BF16)
    cm = sb("cm", [128, 8])
    gbh = sb("gbh", [128, 3, D], BF16)
    gfin = sb("gfin", [128, D])
    gsub = sb("gsub", [128, 128]); grec4 = sb("grec4", [128, 4, 128])
    lamv = sb("lamv", [128, 4])
    lb_b = sb("lb_b", [128, 512]); oml_b = sb("oml_b", [128, 512])
    wrt = sb("wrt", [128, 8, 20]); brt = sb("brt", [128, 20])
    hm = sb("hm", [64, 8]); posf = sb("posf", [128, 1])
    tri8 = sb("tri8", [64, 8])
    idx = sb("idx", [128, NPAGES], I32)
    pti = kout[:, 256:256 + NPAGES].bitcast(I32)
    Zq = sb("Zq", [128, 4, 64], BF16)
    sacc = sb("sacc", [64, 128]); sml = sb("sml", [64, 16])
    onorm = sb("onorm", [64, 128])
    sqT = sb("sqT", [128, 4, 128], BF16); skT = sb("skT", [128, 4, 128], BF16)
    sod = sb("sod", [128, 512], BF16)

    ps = [es.enter_context(nc.psum_tensor("ps%d" % i, [128, 512], F32)) for i in range(8)]
    P = Prog(nc)
    cnt = {"bank": 0, "ring": 0, "ev": 0, "bset": 8, "sl": 0, "inter": False, "hb": 0, "ti": 0}

    def hbank():
        if cnt["inter"]:
            cnt["hb"] = (cnt["hb"] + 1) % 3
            return ps[cnt["hb"]]
        return gbank()

    def gbank():
        if cnt["inter"]:
            cnt["bank"] = (cnt["bank"] + 1) % 3
            return ps[3 + cnt["bank"]]
        cnt["bank"] = (cnt["bank"] + 1) % cnt["bset"]
        return ps[cnt["bank"]]

    def aps(*xs):
        return [x for x in xs if x is not None and not isinstance(x, (int, float))]

    def mm(out, lhsT, rhs, start, stop, **kw):
        P.op("pe", lambda e: e.matmul(out, lhsT=lhsT, rhs=rhs, start=start, stop=stop, **kw), [lhsT, rhs], [out])

    def tr(out, in_, idn):
        P.op("pe", lambda e: e.transpose(out=out, in_=in_, identity=idn), [in_, idn], [out])

    def act(out, in_, func, bias=None, scale=None, accum_out=None, eng="act"):
        kw = {}
        if bias is not None: kw["bias"] = bias
        if scale is not None: kw["scale"] = scale
        if accum_out is not None: kw["accum_out"] = accum_out
        P.op("act", lambda e: e.activation(out=out, in_=in_, func=func, **kw),
             aps(in_, bias, scale), aps(out, accum_out))

    def tt(out, in0, in1, op, eng="dve"):
        P.op(eng, lambda e: e.tensor_tensor(out=out, in0=in0, in1=in1, op=op), [in0, in1], [out])

    def ts(out, in0, s1, s2, op0, op1=None, eng="dve"):
        if op1 is None:
            P.op(eng, lambda e: e.tensor_scalar(out=out, in0=in0, scalar1=s1, scalar2=None, op0=op0), aps(in0, s1), [out])
        else:
            P.op(eng, lambda e: e.tensor_scalar(out=out, in0=in0, scalar1=s1, scalar2=s2, op0=op0, op1=op1),
                 aps(in0, s1, s2), [out])

    def stt(out, in0, scalar, in1, op0, op1, eng="dve"):
        P.op(eng, lambda e: e.scalar_tensor_tensor(out=out, in0=in0, scalar=scalar, in1=in1, op0=op0, op1=op1),
             aps(in0, scalar, in1), [out])

    def cp(out, in_, eng=None):
        if eng is None:
            cnt["ev"] += 1
            eng = "act" if cnt["ev"] % 2 else "dve"
        if eng == "act":
            P.op("act", lambda e: e.copy(out=out, in_=in_), [in_], [out])
        else:
            P.op(eng, lambda e: e.tensor_copy(out=out, in_=in_), [in_], [out])

    def recip(out, in_):
        P.op("dve", lambda e: e.reciprocal(out=out, in_=in_), [in_], [out])

    def memset(ap, v, eng="dve"):
        P.op(eng, lambda e: e.memset(ap, v), [], [ap])

    def rmax(out, in_):
        P.op("dve", lambda e: e.reduce_max(out=out, in_=in_, axis=mybir.AxisListType.X), [in_], [out])

    def rsum(out, in_):
        P.op("dve", lambda e: e.reduce_sum(out=out, in_=in_, axis=mybir.AxisListType.X), [in_], [out])

    def load(out, in_, eng="sp"):
        P.dma(eng, out, in_)

    def store(out, in_, eng="pool"):
        P.dma(eng, out, in_)

    def wslot():
        cnt["ring"] = (cnt["ring"] + 1) % 4
        return ring[:, cnt["ring"], :]

    def rstd_of(src, n, dst):
        act(wk2[:, 0:n], src, AF.Square, accum_out=st4[:, 15:16])
        act(st4[:, 14:15], st4[:, 15:16], AF.Sqrt, bias=EPS, scale=1.0 / n)
        recip(dst, st4[:, 14:15])

    load(ident[:], c_ident); load(Um[:], c_U); load(U2m[:], c_U2); load(wk2[:, 0:256], c_caus); load(cm[:], c_cm)
    cp(identb[:], ident[:], "dve"); cp(causb[:], wk2[:, 0:256], "dve")
    for i in range(3):
        load(wk[:], gains[i].partition_broadcast(128))
        cp(gbh[:, i, :], wk[:], "dve")
    load(gfin[:], gains[3].partition_broadcast(128))
    load(gsub[:], gsmall[0].partition_broadcast(128))
    for h in range(4):
        load(grec4[:, h, :], gsmall[1].partition_broadcast(128))
    ts(gsub[:], gsub[:], float((1.0 - LAM_INIT) * math.sqrt(1.0)), None, ALU.mult)
    lamb = wk2[:, 256:512]
    load(lamb, lam[0].partition_broadcast(128))
    lbb = wk2[:, 0:1024].rearrange("p (a b) -> p a b", a=2)
    load(wrt[:], w_rt.rearrange("(c p) n -> p c n", p=128)); load(brt[:], b_rt[0].partition_broadcast(128))
    load(hm[:], c_hm); load(posf[:], c_pos); load(tri8[:], c_tri8)
    tt(wk[:, 0:64], lamb[:, 0:64], lamb[:, 64:128], ALU.mult)
    rsum(lamv[:, 2:3], wk[:, 0:64])
    tt(wk[:, 64:128], lamb[:, 128:192], lamb[:, 192:256], ALU.mult)
    rsum(lamv[:, 3:4], wk[:, 64:128])
    act(lamv[:, 2:4], lamv[:, 2:4], AF.Exp)
    tt(lamv[:, 0:1], lamv[:, 2:3], lamv[:, 3:4], ALU.subtract)
    ts(lamv[:, 0:1], lamv[:, 0:1], float(LAM_INIT), None, ALU.add)
    ts(lamv[:, 1:2], lamv[:, 0:1], -1.0, None, ALU.mult)
    load(lbb[:, 0, :], lbp[0].partition_broadcast(128)); load(lbb[:, 1, :], lbp[1].partition_broadcast(128))
    tt(lb_b[:], lbb[:, 1, :], lbb[:, 0, :], ALU.subtract)
    act(lb_b[:], lb_b[:], AF.Exp)
    ts(lb_b[:], lb_b[:], 1.0, None, ALU.add)
    recip(lb_b[:], lb_b[:])
    ts(oml_b[:], lb_b[:], -1.0, 1.0, ALU.mult, ALU.add)

    def cast_rows(src, dst, rows):
        n = src.shape[0]
        for r0 in range(0, n, rows):
            P.dma("pool", dst[r0:r0 + rows], src[r0:r0 + rows])

    if STOP >= 1:
        cast_rows(w_in, scr_win, 128)
        cast_rows(w_ba, scr_wba, 512); cast_rows(w_br, scr_wbr, 512); cast_rows(w_out, scr_wout, 512)
        for e in range(16):
            P.dma("pool", scr_wg[e], w_g[e]); P.dma("pool", scr_wu[e], w_u[e]); P.dma("pool", scr_wd[e], w_d[e])
        cast_rows(w_pg, scr_wpg, 512); cast_rows(w_ple, scr_wple, 256)

    KT = KTt[:, :, 0:SEQ]
    hwk = hTf[:].rearrange("p c t -> p (c t)")
    hwkb = merged
    memset(qblk[:], 0.0)
    Vp = Vpt[:, 0:SEQ // 128]
    scale = 0.125

    def stage_in(ti, xrows, prow, sample):
        if sample:
            memset(xt[:, ti, :], 0.0)
            memset(ptile[:], 0.0)
            for b in range(NSAMP):
                load(xt[32 * b:32 * b + TS, ti, :], x_s[b * TS:(b + 1) * TS, :])
        else:
            load(xt[:, ti, :], x_p[xrows:xrows + 128, :])
        rstd_of(xt[:, ti, :], D, st4[:, 0:1])
        stt(wkb[:], xt[:, ti, :], st4[:, 0:1], gbh[:, 0, :], ALU.mult, ALU.mult)
        bank = gbank()
        pb = bank[:].bitcast(BF16)
        for c in range(8):
            tr(pb[:, c * 128:(c + 1) * 128], wkb[:, c * 128:(c + 1) * 128], identb[:])
        cp(xT[:, :, ti * 128:(ti + 1) * 128], pb.rearrange("p (c t) -> p c t", c=8))

    def stage_proj(ntile, jmax=11):
        wv = scr_win.rearrange("(c p) n -> p c n", p=128)
        for j in range(jmax):
            slot = wslot().rearrange("p (c n) -> p c n", c=8)
            load(slot, wv[:, :, j * 512:(j + 1) * 512])
            for ti in range(ntile):
                bank = gbank()
                for c in range(8):
                    mm(bank[:], xT[:, c, ti * 128:(ti + 1) * 128], slot[:, c, :], c == 0, c == 7)
                cp(proj[:, ti, j * 512:(j + 1) * 512], bank[:])

    def rope(dst, src, cs, sn):
        s4 = src.rearrange("p (g two d) -> p g two d", g=8, two=2)
        d4 = dst.rearrange("p (g two d) -> p g two d", g=8, two=2)
        c3 = cs.rearrange("p (g d) -> p g d", g=8); s3 = sn.rearrange("p (g d) -> p g d", g=8)
        t1 = wk[:, 0:256].rearrange("p (g d) -> p g d", g=8)
        t2 = wk[:, 256:512].rearrange("p (g d) -> p g d", g=8)
        t3 = wk[:, 512:768].rearrange("p (g d) -> p g d", g=8)
        t4 = wk[:, 768:1024].rearrange("p (g d) -> p g d", g=8)
        tt(t1, s4[:, :, 0, :], c3, ALU.mult)
        tt(t2, s4[:, :, 1, :], s3, ALU.mult)
        tt(t3, s4[:, :, 1, :], c3, ALU.mult)
        tt(t4, s4[:, :, 0, :], s3, ALU.mult)
        tt(d4[:, :, 0, :], t1, t2, ALU.subtract)
        tt(d4[:, :, 1, :], t3, t4, ALU.add)

    def stage_qkv(ti, orow, posrow, tseq, sample):
        load(cosb[:], c_cos[posrow:posrow + 128, :]); load(sinb[:], c_sin[posrow:posrow + 128, :])
        rope(kout[:], proj[:, ti, OK_:OK_ + 512], cosb[:], sinb[:])
        if sample:
            for b in range(NSAMP):
                store(k_s[b * TS:(b + 1) * TS, :], kout[32 * b:32 * b + TS, :])
                store(v_s[b * TS:(b + 1) * TS, :], proj[32 * b:32 * b + TS, ti, OV:OV + 512])
        else:
            store(k_p[orow:orow + 128, :], kout[:])
            store(v_p[orow:orow + 128, :], proj[:, ti, OV:OV + 512])
        rope(wk2[:, 0:512], proj[:, ti, OQ:OQ + 512], cosb[:], sinb[:])
        cp(wkb[:, 0:512], wk2[:, 0:512]); cp(wkb[:, 512:1024], kout[:])
        bank = gbank(); pb = bank[:].bitcast(BF16)
        for h in range(8):
            tr(pb[:, h * 128:(h + 1) * 128], wkb[:, h * 128:(h + 1) * 128], identb[:])
        cp(qT[:].rearrange("p h t -> p (h t)"), pb[:, 0:512], "act")
        if not sample:
            cp(qblk[0:64, :, 0, :], pb[0:64, 0:512].rearrange("p (h t) -> p h t", h=4), "act")
            cp(qblk[64:128, :, 1, :], pb[64:128, 0:512].rearrange("p (h t) -> p h t", h=4), "act")
        if sample:
            cp(keT[:].rearrange("p h t -> p (h t)"), pb[:, 512:1024])
            return
        if "noKT" not in DBG:
            if "ktalt" in DBG:
                for h in range(4):
                    cp(merged[:, h * 128:(h + 1) * 128], pb[:, (4 + h) * 128:(5 + h) * 128], "act")
            elif "kt2d" in DBG:
                for h in range(4):
                    cp(KT[:, h, tseq * 128:(tseq + 1) * 128], pb[:, (4 + h) * 128:(5 + h) * 128], "act")
            elif "ktlow" in DBG:
                cp(KT[:, :, tseq * 128:(tseq + 1) * 128], pb[:, 0:512].rearrange("p (h t) -> p h t", h=4), "act")
            else:
                cp(KT[:, :, tseq * 128:(tseq + 1) * 128], pb[:, 512:1024].rearrange("p (h t) -> p h t", h=4),
                   "dve" if "ktdve" in DBG else ("act" if "ktact" in DBG else None))
        if "noVp" not in DBG:
            cp(Vp[:, tseq, :, 0:128], proj[:, ti, OV:OV + 512].rearrange("p (h e) -> p h e", h=4),
               "dve" if "vpdve" in DBG else "act")

    def attn_finish(odv_src0, odv_src1, h, rec2):
        ts(st4[:, 4:5], rec2[1], lamv[:, 1:2], None, ALU.mult)
        ts(wk[:, 0:128], odv_src0, rec2[0], None, ALU.mult)
        stt(od[:, h * 128:(h + 1) * 128], odv_src1, st4[:, 4:5], wk[:, 0:128], ALU.mult, ALU.add)

    def subln_to_yaT():
        for h in range(4):
            act(wk2[:, 0:128], od[:, h * 128:(h + 1) * 128], AF.Square, accum_out=st4[:, 8 + h:9 + h])
        act(st4[:, 8:12], st4[:, 8:12], AF.Sqrt, bias=EPS, scale=1.0 / 128)
        recip(st4[:, 8:12], st4[:, 8:12])
        for h in range(4):
            stt(wkb[:, h * 128:(h + 1) * 128], od[:, h * 128:(h + 1) * 128], st4[:, 8 + h:9 + h], gsub[:], ALU.mult, ALU.mult)
        bank = gbank(); pb = bank[:].bitcast(BF16)
        for h in range(4):
            tr(pb[:, h * 128:(h + 1) * 128], wkb[:, h * 128:(h + 1) * 128], identb[:])
        cp(yaT2[:, cnt["ti"], :, :].rearrange("p h t -> p (h t)"), pb[:, 0:512])

    def stage_attn_prompt(tseq):
        cnt["bset"] = 2
        nk = tseq + 1
        steps = []
        for h in range(4):
            i = 0
            while i < nk:
                n = 2 if i + 1 < nk else 1
                steps.append((h, i, n)); i += n
        if cnt["inter"]:
            sbanks = [ps[3], ps[4], ps[5]]
            accs = [(ps[6], ps[7]), (ps[6], ps[7])]
        else:
            sbanks = [ps[2], ps[3], ps[4], ps[5]]
            accs = [(ps[6], ps[7]), (ps[0], ps[1])]
        nsb = len(sbanks)
        LA = 2

        def S_E(k):
            h, i0, n = steps[k]
            bank = sbanks[k % nsb]
            for u in range(n):
                mm(bank[:, u * 256:(u + 1) * 256], KT[:, h, (i0 + u) * 128:(i0 + u + 1) * 128],
                   qblk[:, h, :, :].rearrange("p c t -> p (c t)"), True, True)
            pt = pT4[:, k % 4, 0:n * 256]
            act(pt, bank[:, 0:n * 256], AF.Exp, scale=scale)
            if i0 + n - 1 == tseq:
                pd = pT4[:, k % 4, (n - 1) * 256:n * 256]
                tt(pd, pd, causb[:], ALU.mult)

        def PV(k):
            h, i0, n = steps[k]
            a0, a1 = accs[h % 2]
            for u in range(n):
                i = i0 + u
                for c, ab in enumerate((a0, a1)):
                    mm(ab[:, 0:130], pT4[:, k % 4, u * 256 + c * 128:u * 256 + (c + 1) * 128], Vp[:, i, h, :],
                       i == 0, i == tseq)
            if i0 + n - 1 == tseq:
                recip(st4[:, 5:6], a0[:, 128:129]); recip(st4[:, 6:7], a1[:, 128:129])
                attn_finish(a0[:, 0:128], a1[:, 0:128], h, (st4[:, 5:6], st4[:, 6:7]))

        for k in range(len(steps) + LA):
            if k < len(steps):
                S_E(k)
            if k - LA >= 0:
                PV(k - LA)
            yield
        subln_to_yaT()
        cnt["bset"] = 8
        yield

    def stage_hgrn(ti, sample, first, last, seq):
        pj = proj[:, ti, :]
        act(hwk[:, 0:512], pj[:, OFR:OFR + 512], AF.Sigmoid)
        tt(hwk[:, 0:512], hwk[:, 0:512], oml_b[:], ALU.mult)
        tt(hwk[:, 0:512], hwk[:, 0:512], lb_b[:], ALU.add)
        act(gbuf[:], hwk[:, 0:512], AF.Ln)
        ts(kbuf[:], hwk[:, 0:512], -1.0, 1.0, ALU.mult, ALU.add)
        yield
        if sample:
            ts(gbuf[:], gbuf[:], cm[:, 4:5], None, ALU.mult)
            ts(kbuf[:], kbuf[:], cm[:, 4:5], None, ALU.mult)
        bank = hbank()
        mm(bank[:], U2m[:], gbuf[:], True, True)
        act(hwk[:, 512:1024], bank[:], AF.Exp)
        tt(kd[:], kbuf[:], hwk[:, 512:1024], ALU.mult)
        yield
        cp(vb[:], pj[:, OIR:OIR + 512])
        vsrc = pj[:, OIR:OIR + 512].rearrange("p (h e) -> p h e", h=4)
        for c in range(4):
            ts(vblk[:, :, c, :], vsrc, cm[:, c:c + 1], None, ALU.mult)
            yield
        bank = hbank()
        for h in range(4):
            mm(bank[:, h * 128:(h + 1) * 128], gbuf[:, h * 128:(h + 1) * 128], Um[:], True, True)
        act(ebT[:], bank[:], AF.Exp)
        act(enbT[:], bank[:], AF.Exp, scale=-1.0)
        yield
        cp(dec[:].rearrange("p (h c) -> p h c", h=4),
           ebT[:].rearrange("p (h c j) -> p h c j", h=4, c=4)[:, :, :, 31], "dve")
        act(hwkb[:, 0:512], pj[:, OQR:OQR + 512], AF.Silu)
        cp(hwkb[:, 512:1024], kbuf[:])
        yield
        bank = hbank(); pb = bank[:].bitcast(BF16)
        for h in range(8):
            tr(pb[:, h * 128:(h + 1) * 128], hwkb[:, h * 128:(h + 1) * 128], identb[:])
        tt(qeT[:].rearrange("p h t -> p (h t)"), pb[:, 0:512], ebT[:], ALU.mult)
        tt(keT[:].rearrange("p h t -> p (h t)"), pb[:, 512:1024], enbT[:], ALU.mult)
        yield
        bank = hbank()
        for h in range(4):
            mm(bank[:, h * 128:(h + 1) * 128], keT[:, h, :], qeT[:, h, :], True, True)
        for h in range(4):
            tt(ATm[:, h, :], bank[:, h * 128:(h + 1) * 128], Um[:], ALU.mult)
            yield
        if (not sample) and first:
            memset(Sst[:, 0, :, :], 0.0)
        for h in range(4):
            bank = hbank()
            mm(bank[:], kd[:, h * 128:(h + 1) * 128], vblk[:, h, :, :].rearrange("p c v -> p (c v)"), True, True)
            for c in range(4):
                sl = Sst[:, (c % 2) if sample else 0, h, :]
                if sample:
                    load(sl, state0[c, h])
                cp(Sb[:, h, c, :], sl, "act")
                stt(sl, sl, dec[:, h * 4 + c:h * 4 + c + 1], bank[:, c * 128:(c + 1) * 128], ALU.mult, ALU.add)
                if sample:
                    store(s_s[c, h], sl)
            yield
        if (not sample) and last:
            store(s_p[seq].rearrange("h k v -> k h v"), Sst[:, 0, :, :])
        obank = hbank()
        for h in range(4):
            mm(obank[:, h * 128:(h + 1) * 128], ATm[:, h, :], vb[:, h * 128:(h + 1) * 128], True, False)
            for c in range(4):
                mm(obank[32 * c:32 * c + 32, h * 128:(h + 1) * 128], qeT[:, h, 32 * c:32 * c + 32], Sb[:, h, c, :],
                   False, True, tile_position=(0, 32 * c))
            yield
        for h in range(4):
            act(ptile[:, 0:128], obank[:, h * 128:(h + 1) * 128], AF.Square, accum_out=st4[:, 16 + h:17 + h])
        act(st4[:, 16:20], st4[:, 16:20], AF.Sqrt, bias=EPS, scale=1.0 / 128)
        recip(st4[:, 16:20], st4[:, 16:20])
        yield
        act(hwk[:, 0:512], pj[:, OGR:OGR + 512], AF.Silu)
        tt(hwk[:, 0:512], hwk[:, 0:512], grec4[:].rearrange("p h e -> p (h e)"), ALU.mult)
        for h in range(4):
            stt(hwkb[:, h * 128:(h + 1) * 128], obank[:, h * 128:(h + 1) * 128], st4[:, 16 + h:17 + h],
                hwk[:, h * 128:(h + 1) * 128], ALU.mult, ALU.mult)
        bank = hbank(); pb = bank[:].bitcast(BF16)
        for h in range(4):
            tr(pb[:, h * 128:(h + 1) * 128], hwkb[:, h * 128:(h + 1) * 128], identb[:])
        cp(yrT2[:, ti, :, :].rearrange("p h t -> p (h t)"), pb[:, 0:512])
        yield

    def stage_merge_tile(ti, Wa, Wr):
        pj = proj[:, ti, :]
        for half in range(2):
            cs = slice(half * 512, (half + 1) * 512)
            ba = gbank()
            for c in range(4):
                mm(ba[:], yaT2[:, ti, c, :], Wa[:, c, cs], c == 0, c == 3)
            br = gbank()
            for c in range(4):
                mm(br[:], yrT2[:, ti, c, :], Wr[:, c, cs], c == 0, c == 3)
            act(wk[:, 0:512], pj[:, OGA + half * 512:OGA + (half + 1) * 512], AF.Sigmoid)
            act(wk[:, 512:1024], pj[:, OGRR + half * 512:OGRR + (half + 1) * 512], AF.Sigmoid)
            tt(wk[:, 0:512], wk[:, 0:512], ba[:], ALU.mult)
            tt(wk[:, 512:1024], wk[:, 512:1024], br[:], ALU.mult)
            tt(merged[:, cs], wk[:, 0:512], wk[:, 512:1024], ALU.add)
        bank = gbank(); pb = bank[:].bitcast(BF16)
        for c in range(8):
            tr(pb[:, c * 128:(c + 1) * 128], merged[:, c * 128:(c + 1) * 128], identb[:])
        cp(xT[:, :, ti * 128:(ti + 1) * 128], pb.rearrange("p (c t) -> p c t", c=8))

    def lin_accum(ntile, wview, src_T):
        wv = wview.rearrange("(c p) n -> p c n", p=128)
        for half in range(2):
            slot = wslot().rearrange("p (c n) -> p c n", c=8)
            load(slot, wv[:, :, half * 512:(half + 1) * 512])
            for ti in range(ntile):
                bank = gbank()
                for c in range(8):
                    mm(bank[:], src_T[:, c, ti * 128:(ti + 1) * 128], slot[:, c, :], c == 0, c == 7)
                tt(xt[:, ti, half * 512:(half + 1) * 512], xt[:, ti, half * 512:(half + 1) * 512], bank[:], ALU.add)

    def stage_route(ti):
        rstd_of(xt[:, ti, :], D, st4[:, 0:1])
        stt(wk[:], xt[:, ti, :], st4[:, 0:1], gbh[:, 1, :], ALU.mult, ALU.mult)
        for half in range(2):
            bank = gbank()
            for c in range(4):
                tr(bank[:, c * 128:(c + 1) * 128], wk[:, (half * 4 + c) * 128:(half * 4 + c + 1) * 128], ident[:])
            cp(hTf[:, half * 4:(half + 1) * 4, :], bank[:].rearrange("p (c t) -> p c t", c=4))
            cp(xT[:, half * 4:(half + 1) * 4, ti * 128:(ti + 1) * 128], bank[:].rearrange("p (c t) -> p c t", c=4))
        bank = gbank()
        for c in range(8):
            mm(bank[:, 0:20], hTf[:, c, :], wrt[:, c, :], c == 0, c == 7)
        lg = rt[:, 0:20]
        tt(lg, bank[:, 0:20], brt[:], ALU.add)
        rmax(rt[:, 20:21], rt[:, 0:4])
        ts(rt[:, 21:22], rt[:, 20:21], -1.0, None, ALU.mult)
        act(rt[:, 24:28], rt[:, 0:4], AF.Exp, bias=rt[:, 21:22], accum_out=rt[:, 22:23])
        recip(rt[:, 23:24], rt[:, 22:23])
        ts(rt[:, 24:28], rt[:, 0:4], rt[:, 20:21], None, ALU.is_ge)
        ts(rt[:, 28:32], rt[:, 24:28], 1e30, -1e30, ALU.mult, ALU.add)
        le = rt[:, 32:48]
        for g in range(4):
            ts(rt[:, 32 + 4 * g:36 + 4 * g], rt[:, 4 + 4 * g:8 + 4 * g], rt[:, 28 + g:29 + g], None, ALU.add)
        rmax(rt[:, 48:49], le)
        m1 = cmb[:, ti, :]
        ts(m1, le, rt[:, 48:49], None, ALU.is_ge)
        stt(wk2[:, 0:16], m1, -1e30, le, ALU.mult, ALU.add)
        rmax(rt[:, 49:50], wk2[:, 0:16])
        ts(wk2[:, 16:32], wk2[:, 0:16], rt[:, 49:50], None, ALU.is_ge)
        tt(rt[:, 50:51], rt[:, 49:50], rt[:, 48:49], ALU.subtract)
        act(rt[:, 50:51], rt[:, 50:51], AF.Exp)
        ts(rt[:, 50:51], rt[:, 50:51], 1.0, None, ALU.add)
        recip(rt[:, 51:52], rt[:, 50:51])
        ts(rt[:, 52:53], rt[:, 51:52], -1.0, 1.0, ALU.mult, ALU.add)
        tt(rt[:, 51:52], rt[:, 51:52], rt[:, 23:24], ALU.mult)
        tt(rt[:, 52:53], rt[:, 52:53], rt[:, 23:24], ALU.mult)
        ts(m1, m1, rt[:, 51:52], None, ALU.mult)
        stt(m1, wk2[:, 16:32], rt[:, 52:53], m1, ALU.mult, ALU.add)

    def stage_experts(ntile):
        ntok = ntile * 128
        bgon = bgs["gen"] is not None
        if bgon:
            cnt["bset"] = 6
        bgs["wdone"] = False
        for e in range(16):
            if bgon:
                bg_tick()
            s1 = wslot()
            gv = s1[:, 0:2048].rearrange("p (c f) -> p c f", c=8); uv = s1[:, 2048:4096].rearrange("p (c f) -> p c f", c=8)
            load(gv, scr_wg[e].rearrange("(c p) f -> p c f", p=128))
            load(uv, scr_wu[e].rearrange("(c p) f -> p c f", p=128))
            s2 = wslot()
            dv = s2[:, 0:2048].rearrange("p (c n) -> p c n", c=2)
            load(dv, scr_wd[e].rearrange("(c p) n -> p c n", p=128))
            for fc in range(2):
                bank = gbank()
                for c in range(8):
                    mm(bank[:, 0:ntok], gv[:, c, fc * 128:(fc + 1) * 128], xT[:, c, 0:ntok], c == 0, c == 7)
                for c in range(8):
                    mm(bank[:, 256:256 + ntok], uv[:, c, fc * 128:(fc + 1) * 128], xT[:, c, 0:ntok], c == 0, c == 7)
                cnt["sl"] = (cnt["sl"] + 1) % 4
                sgs = wk2[:, cnt["sl"] * 256:cnt["sl"] * 256 + ntok]
                act(sgs, bank[:, 0:ntok], AF.Silu)
                tt(hid[:, e % 2, fc, 0:ntok], sgs, bank[:, 256:256 + ntok], ALU.mult)
                if bgon:
                    bg_tick()
            for ti in range(ntile):
                for half in range(2):
                    bank = gbank()
                    for fc in range(2):
                        mm(bank[:], hid[:, e % 2, fc, ti * 128:(ti + 1) * 128], dv[:, fc, half * 512:(half + 1) * 512], fc == 0, fc == 1)
                    xs = xt[:, ti, half * 512:(half + 1) * 512]
                    stt(xs, bank[:], cmb[:, ti, e:e + 1], xs, ALU.mult, ALU.add)
                    if bgon:
                        bg_tick()
        if bgon:
            bg_tick(drain=True)
        cnt["bset"] = 8

    def stage_ple_prep(ti, prow, sample):
        rstd_of(xt[:, ti, :], D, st4[:, 0:1])
        stt(wkb[:], xt[:, ti, :], st4[:, 0:1], gbh[:, 2, :], ALU.mult, ALU.mult)
        bank = gbank(); pb = bank[:].bitcast(BF16)
        for c in range(8):
            tr(pb[:, c * 128:(c + 1) * 128], wkb[:, c * 128:(c + 1) * 128], identb[:])
        cp(xT[:, :, ti * 128:(ti + 1) * 128], pb.rearrange("p (c t) -> p c t", c=8))
        if sample:
            for b in range(NSAMP):
                load(ptile[32 * b:32 * b + TS, :], p_s[b * TS:(b + 1) * TS, :])
        else:
            load(ptile[:], p_p[prow:prow + 128, :])
        cp(merged[:, 0:256], ptile[:])
        bank = gbank(); pb = bank[:].bitcast(BF16)
        for c in range(2):
            tr(pb[:, c * 128:(c + 1) * 128], merged[:, c * 128:(c + 1) * 128], identb[:])
        cp(ppT[:, :, ti * 128:(ti + 1) * 128], pb[:, 0:256].rearrange("p (c t) -> p c t", c=2))

    def stage_ple(ntile):
        s3 = wslot()
        wp = s3[:, 0:2048].rearrange("p (c n) -> p c n", c=2)
        load(wp, scr_wple.rearrange("(c p) n -> p c n", p=128))
        wv = scr_wpg.rearrange("(c p) n -> p c n", p=128)
        for half in range(2):
            slot = wslot().rearrange("p (c n) -> p c n", c=8)
            load(slot, wv[:, :, half * 512:(half + 1) * 512])
            for ti in range(ntile):
                b1 = gbank()
                for c in range(8):
                    mm(b1[:], xT[:, c, ti * 128:(ti + 1) * 128], slot[:, c, :], c == 0, c == 7)
                b2 = gbank()
                for c in range(2):
                    mm(b2[:], ppT[:, c, ti * 128:(ti + 1) * 128], wp[:, c, half * 512:(half + 1) * 512], c == 0, c == 1)
                act(wk[:, 0:512], b1[:], AF.Sigmoid)
                tt(wk[:, 0:512], wk[:, 0:512], b2[:], ALU.mult)
                xs = xt[:, ti, half * 512:(half + 1) * 512]
                tt(xs, xs, wk[:, 0:512], ALU.add)

    def stage_final(ti, orow, sample):
        rstd_of(xt[:, ti, :], D, st4[:, 0:1])
        stt(wk[:], xt[:, ti, :], st4[:, 0:1], gfin[:], ALU.mult, ALU.mult)
        if sample:
            for b in range(NSAMP):
                store(y_s[b * TS:(b + 1) * TS, :], wk[32 * b:32 * b + TS, :])
        else:
            store(y_p[orow:orow + 128, :], wk[:])

    kpg = KTt[:].rearrange("p h t -> p (h t)")[:, 0:8192].rearrange("p (b j n) -> p b j n", b=4, j=4)
    vpg = Vpt[:].rearrange("p t h e -> p (t h e)")[:, 0:8192].rearrange("p (b j n) -> p b j n", b=4, j=4)
    _unused = proj[0:64, 1, 0:8]
    kTs = wk
    GP = 8

    projflat = proj[:].rearrange("p a b -> p (a b)")
    kpgB = projflat[:, 0:4096].bitcast(BF16).rearrange("p (b j n) -> p b j n", b=4, j=4)
    vpgB = projflat[:, 4096:8192].bitcast(BF16).rearrange("p (b j n) -> p b j n", b=4, j=4)
    scB = [projflat[0:64, 8192:9224], projflat[0:64, 9224:10256]]
    selB = projflat[0:64, 10256:10768]
    NQ = 2
    NG = NPAGES // 8

    def sgather(dst, src, ix):
        P.dma("pool", dst, src, extra_reads=[ix],
              fn=lambda e: e.indirect_dma_start(out=dst, out_offset=None, in_=src,
                                                in_offset=bass.IndirectOffsetOnAxis(ap=ix, axis=0)))

    def bg_setup(b):
        load(pti, ptab[b].partition_broadcast(128))
        cp(kout[:, 0:NPAGES], pti, "dve")
        ts(kout[:, 0:NPAGES], kout[:, 0:NPAGES], 128.0, posf[:, 0:1], ALU.mult, ALU.add)
        cp(idx[:], kout[:, 0:NPAGES], "dve")
        memset(Zq[:], 0.0)
        for h in range(4):
            for c in range(2):
                cp(Zq[c * 64:(c + 1) * 64, h, h * 16 + c * 8:h * 16 + c * 8 + 8],
                   sqT[c * 64:(c + 1) * 64, h, 32 * b:32 * b + TS], "dve")
        memset(sml[:, 0:1], -1e30); memset(sml[:, 1:2], 0.0); memset(sacc[:], 0.0)

    def bg_gather(u, slot):
        b, g = u
        for q in range(NQ):
            pg0 = g * 8 + q * 4
            for j in range(4):
                sgather(kpgB[:, q + NQ * slot, j, :], cache_k, idx[:, pg0 + j:pg0 + j + 1])
            yield
        for q in range(NQ):
            pg0 = g * 8 + q * 4
            for j in range(4):
                sgather(vpgB[:, q + NQ * slot, j, :], cache_v, idx[:, pg0 + j:pg0 + j + 1])
            yield

    def bg_softmax(sc, ncols):
        scg = sc[:, 0:ncols]
        m_run = sml[:, 0:1]; l_run = sml[:, 1:2]
        rmax(sml[:, 2:3], scg)
        yield
        tt(sml[:, 3:4], sml[:, 2:3], m_run, ALU.max)
        yield
        tt(sml[:, 4:5], m_run, sml[:, 3:4], ALU.subtract)
        ts(sml[:, 5:6], sml[:, 3:4], -scale, None, ALU.mult)
        yield
        act(sml[:, 4:5], sml[:, 4:5], AF.Exp, scale=scale)
        act(scg, scg, AF.Exp, bias=sml[:, 5:6], scale=scale, accum_out=sml[:, 6:7])
        yield
        stt(l_run, l_run, sml[:, 4:5], sml[:, 6:7], ALU.mult, ALU.add)
        cp(m_run, sml[:, 3:4], "dve")
        yield

    def bg_acc():
        ts(sacc[:], sacc[:], sml[:, 4:5], None, ALU.mult)
        yield
        for h in range(4):
            stt(sacc[:], ps[7][0:64, h * 128:(h + 1) * 128], hm[:, h:h + 1], sacc[:], ALU.mult, ALU.add)
            if h % 2 == 1:
                yield

    def bg_kpart(u, slot):
        b, g = u
        sc = scB[slot]
        for q in range(NQ):
            qb = q + NQ * slot
            for h in range(4):
                tb = gbank(); tbb = tb[:].bitcast(BF16)
                for j in range(4):
                    tr(tbb[:, j * 128:(j + 1) * 128], kpgB[:, qb, j, h * 128:(h + 1) * 128], identb[:])
                kts = wkb[:, (h % 2) * 512:(h % 2 + 1) * 512]
                yield
                cp(kts, tbb[:, 0:512], "act")
                yield
                mm(ps[6][0:64, :], Zq[:, h, :], kts, h == 0, h == 3)
            yield
            cp(sc[:, q * 512:(q + 1) * 512], ps[6][0:64, :], "act")
        yield
        yield from bg_softmax(sc, NQ * 512)

    def bg_pvpart(u, slot):
        b, g = u
        sc = scB[slot]
        for q in range(NQ):
            tb = gbank()
            for j in range(4):
                tr(tb[:, j * 64:(j + 1) * 64], sc[:, (q * 4 + j) * 128:(q * 4 + j + 1) * 128], ident[0:64, 0:64])
            ptb = pT4[:, q % 2, 0:256]
            yield
            cp(ptb, tb[:, 0:256], "act")
            yield
            for j in range(4):
                mm(ps[7][0:64, :], ptb[:, j * 64:(j + 1) * 64], vpgB[:, q + NQ * slot, j, :],
                   q == 0 and j == 0, q == NQ - 1 and j == 3)
            yield
        yield from bg_acc()

    def bg_final(b):
        sc = scB[0]
        for h in range(4):
            mm(ps[6][0:64, 0:8], Zq[:, h, :], skT[:, h, 32 * b:32 * b + TS], h == 0, h == 3)
        ts(kout[0:64, 256:264], tri8[:], 1e30, -1e30, ALU.mult, ALU.add)
        tt(sc[:, 0:8], ps[6][0:64, 0:8], kout[0:64, 256:264], ALU.add)
        yield from bg_softmax(sc, 8)
        tb = gbank()
        tr(tb[0:8, 0:64], sc[:, 0:8], ident[0:64, 0:64])
        cp(kout[0:8, 384:448], tb[0:8, 0:64])
        load(kbuf[0:8, :], v_s[b * TS:(b + 1) * TS, :])
        yield
        mm(ps[7][0:64, :], kout[0:8, 384:448], kbuf[0:8, :], True, True)
        yield from bg_acc()
        recip(sml[:, 7:8], sml[:, 1:2])
        stt(sml[:, 8:9], hm[:, 5:6], lamv[0:64, 1:2], hm[:, 4:5], ALU.mult, ALU.add)
        tt(sml[:, 8:9], sml[:, 8:9], sml[:, 7:8], ALU.mult)
        ts(onorm[:], sacc[:], sml[:, 8:9], None, ALU.mult)
        load(selB, c_sel[b])
        tb = gbank()
        for h in range(4):
            mm(tb[:, h * 128:(h + 1) * 128], selB[:, h * 128:(h + 1) * 128], onorm[:], True, True)
        if b == 0:
            cp(sod[:], tb[:], "dve")
        else:
            tt(sod[:], sod[:], tb[:], ALU.add)
        yield

    def bg_generator():
        units = [(b, g) for b in range(NSAMP) for g in range(NG)]
        for w0 in range(0, len(units), 4):
            win = units[w0:w0 + 4]
            n = len(win)
            for step in range(n + 2):
                if step - 2 >= 0:
                    u = win[step - 2]
                    yield from bg_pvpart(u, (step - 2) % 2)
                    if u[1] == NG - 1:
                        yield from bg_final(u[0])
                if step < n:
                    u = win[step]
                    if u[1] == 0:
                        bg_setup(u[0])
                        yield
                    yield from bg_gather(u, step % 2)
                if 0 <= step - 1 < n:
                    yield from bg_kpart(win[step - 1], (step - 1) % 2)
            yield "WINDOW_END"

    bgs = {"gen": None, "wdone": False}

    def bg_tick(drain=False):
        g = bgs["gen"]
        if g is None or bgs["wdone"]:
            return
        try:
            k = 0
            while True:
                r = next(g)
                k += 1
                if r == "WINDOW_END":
                    bgs["wdone"] = True
                    return
                if not drain and k >= 2:
                    return
        except StopIteration:
            bgs["gen"] = None

    def macro(tiles, sample):
        ntile = len(tiles)
        if sample:
            while bgs["gen"] is not None:
                bgs["wdone"] = False
                bg_tick(drain=True)
        if STOP < 2:
            return
        for ti, tinfo in enumerate(tiles):
            stage_in(ti, tinfo["row"], tinfo["row"], sample)
        stage_proj(ntile)
        if STOP < 3:
            return
        def run(g):
            for _ in g:
                pass

        def hg(ti):
            tinfo = tiles[ti]
            return stage_hgrn(ti, sample, tinfo["tseq"] == 0, tinfo["tseq"] == SEQ // 128 - 1, tinfo["seq"])

        pend = None
        for ti, tinfo in enumerate(tiles):
            cnt["ti"] = ti
            if sample:
                cp(od[:], sod[:], "dve")
                subln_to_yaT()
                run(hg(ti))
                continue
            stage_qkv(ti, tinfo["row"], tinfo["pos"], tinfo["tseq"], sample)
            if pend is None:
                run(stage_attn_prompt(tinfo["tseq"]))
            else:
                cnt["inter"] = True
                ga = stage_attn_prompt(tinfo["tseq"]); gh = pend
                alive = [ga, gh]
                while alive:
                    for g in list(alive):
                        try:
                            next(g)
                        except StopIteration:
                            alive.remove(g)
                cnt["inter"] = False
            pend = hg(ti)
        if pend is not None and not sample:
            run(pend)
        for ti, tinfo in enumerate(tiles):
            if ti == 0:
                Wa = wslot().rearrange("p (c n) -> p c n", c=4)
                load(Wa, scr_wba.rearrange("(c p) n -> p c n", p=128))
                Wr = wslot().rearrange("p (c n) -> p c n", c=4)
                load(Wr, scr_wbr.rearrange("(c p) n -> p c n", p=128))
            stage_merge_tile(ti, Wa, Wr)
        if STOP < 6:
            return
        lin_accum(ntile, scr_wout, xT)
        if STOP < 7:
            return
        for ti in range(ntile):
            stage_route(ti)
        if STOP < 8:
            return
        stage_experts(ntile)
        if STOP < 9:
            return
        for ti, tinfo in enumerate(tiles):
            stage_ple_prep(ti, tinfo["row"], sample)
        stage_ple(ntile)
        for ti, tinfo in enumerate(tiles):
            stage_final(ti, tinfo["row"], sample)

    def prepass():
        stage_in(0, 0, 0, True)
        stage_proj(1, jmax=3)
        stage_qkv(0, 0, SEQ, 0, True)
        cp(sqT[:].rearrange("p h t -> p (h t)"), qT[:].rearrange("p h t -> p (h t)"), "dve")
        cp(skT[:].rearrange("p h t -> p (h t)"), keT[:].rearrange("p h t -> p (h t)"), "dve")
        bgs["gen"] = bg_generator()

    return nc, es, P, macro, dict(Vp=Vp, memset=memset, prepass=prepass)


_CACHE = {}


def _get_program():
    if "nc" in _CACHE:
        return _CACHE["nc"]
    nc, es, P, macro, aux = build_program()
    with es:
        aux["memset"](aux["Vp"], 1.0)
        if DO_SAMPLE:
            aux["prepass"]()
        for seq in range(NSEQ if DO_PROMPT else 0):
            for mt in range(SEQ // 256):
                tiles = []
                for k in range(2):
                    tseq = mt * 2 + k
                    tiles.append(dict(row=seq * SEQ + tseq * 128, pos=tseq * 128, tseq=tseq, seq=seq))
                macro(tiles, False)
        if DO_SAMPLE:
            macro([dict(row=0, pos=SEQ, tseq=0, seq=0)], True)
        P.emit()
    _CACHE["nc"] = nc
    _CACHE["stats"] = P.stats
    return nc


def _consts():
    f = np.float32
    ident = np.eye(128, dtype=f)
    s = np.arange(128)[:, None]; t = np.arange(128)[None, :]
    same = (s // 32) == (t // 32)
    U = (same & (s <= t)).astype(f)
    U2 = (same & (s > t)).astype(f)
    caus1 = (s <= t).astype(f)
    caus = np.concatenate([caus1, caus1], axis=1)
    half = 32
    inv_freq = (np.float32(10000.0) ** (-np.arange(half, dtype=f) / np.float32(half))).astype(f)
    pos = np.concatenate([np.arange(SEQ), np.zeros(128)]).astype(f)
    for b in range(NSAMP):
        for tt_ in range(32):
            pos[SEQ + 32 * b + tt_] = PAST + min(tt_, TS - 1)
    ang = (pos[:, None] * inv_freq[None, :]).astype(f)
    cos = np.tile(np.cos(ang).astype(f), (1, 8)); sin = np.tile(np.sin(ang).astype(f), (1, 8))
    cm = np.zeros((128, 8), f)
    for c in range(4):
        cm[32 * c:32 * c + 32, c] = 1
    for b in range(NSAMP):
        cm[32 * b:32 * b + TS, 4] = 1
    sel = np.zeros((64, 16, 128), f)
    hm = np.zeros((64, 8), f)
    tri8 = np.zeros((64, 8), f)
    for h in range(4):
        for c in range(2):
            for tq in range(TS):
                r = h * 16 + c * 8 + tq
                hm[r, h] = 1
                hm[r, 4 + c] = 1
                tri8[r, :tq + 1] = 1
                for b in range(NSAMP):
                    sel[r, b * 4 + h, 32 * b + tq] = 1
    posi = np.arange(128, dtype=np.float32)[:, None]
    return dict(c_ident=ident, c_U=U, c_U2=U2, c_caus=caus, c_cos=np.ascontiguousarray(cos),
                c_sin=np.ascontiguousarray(sin), c_cm=cm, c_sel=np.ascontiguousarray(sel.reshape(64, 4, 512).transpose(1, 0, 2)), c_hm=hm, c_pos=posi,
                c_tri8=tri8)


def _run(inputs, ncores):
    (x_prompt, x_sample, p_prompt, p_sample, cache_k, cache_v, state_hgrn, page_table,
     g_mix, w_in, lam, g_subln, lb_param, g_rec, w_branch_a, w_branch_r, w_out, g_ffn,
     w_route_group, b_route_group, w_route_expert, b_route_expert, w_exp_gate, w_exp_up,
     w_exp_down, g_ple, w_ple_gate, w_ple, g_final) = inputs
    A = lambda a: np.ascontiguousarray(np.asarray(a))
    nc = _get_program()
    consts = _consts()
    shared = dict(
        cache_k=A(cache_k).reshape(NPHYS * PAGE, 512), cache_v=A(cache_v).reshape(NPHYS * PAGE, 512),
        w_in=A(w_in)[0], w_ba=A(w_branch_a)[0], w_br=A(w_branch_r)[0], w_out=A(w_out)[0],
        w_g=A(w_exp_gate)[0], w_u=A(w_exp_up)[0], w_d=A(w_exp_down)[0], w_pg=A(w_ple_gate)[0], w_ple=A(w_ple)[0],
        w_rt=A(np.concatenate([np.asarray(w_route_group)[0], np.asarray(w_route_expert)[0]], axis=1)),
        b_rt=A(np.concatenate([np.asarray(b_route_group)[0], np.asarray(b_route_expert)[0]])[None, :]),
        gains=A(np.stack([np.asarray(g_mix)[0], np.asarray(g_ffn)[0], np.asarray(g_ple)[0], np.asarray(g_final)])),
        gsmall=A(np.stack([np.asarray(g_subln)[0], np.asarray(g_rec)[0]])),
        lam=A(lam).reshape(1, 256), lbp=A(lb_param), **consts)
    xp = A(x_prompt); xs = A(x_sample); pp = A(p_prompt)[0]; psm = A(p_sample)[0]
    st = A(state_hgrn)[0]; pt = A(page_table).astype(np.int32)
    in_maps = []
    for i in range(ncores):
        m = dict(shared)
        m["x_p"] = xp[2 * i:2 * i + 2].reshape(NSEQ * SEQ, D)
        m["p_p"] = np.ascontiguousarray(pp[2 * i:2 * i + 2]).reshape(NSEQ * SEQ, 256)
        m["x_s"] = xs[4 * i:4 * i + 4].reshape(NSAMP * TS, D)
        m["p_s"] = np.ascontiguousarray(psm[4 * i:4 * i + 4]).reshape(NSAMP * TS, 256)
        m["state0"] = np.ascontiguousarray(st[4 * i:4 * i + 4])
        m["ptab"] = np.ascontiguousarray(pt[4 * i:4 * i + 4])
        in_maps.append(m)
    res = run_bass_kernel_spmd(nc, in_maps, core_ids=list(range(ncores)))
    R = res.results
    cat = lambda k: np.concatenate([np.asarray(r[k]) for r in R], axis=0)
    nb = 2 * ncores; ns = 4 * ncores
    y_prompt = cat("y_p").reshape(nb, SEQ, D)
    y_sample = cat("y_s").reshape(ns, TS, D)
    k_prompt = cat("k_p").reshape(1, nb, SEQ, 4, 2, 64)
    v_prompt = cat("v_p").reshape(1, nb, SEQ, 4, 128)
    s_prompt = cat("s_p").reshape(1, nb, 4, 128, 128)
    k_sample = cat("k_s").reshape(1, ns, TS, 4, 2, 64)
    v_sample = cat("v_s").reshape(1, ns, TS, 4, 128)
    s_sample = cat("s_s").reshape(1, ns, 4, 128, 128)
    return (y_prompt, y_sample, k_prompt, v_prompt, s_prompt, k_sample, v_sample, s_sample)


def kernel(x_prompt, x_sample, p_prompt, p_sample, cache_k, cache_v, state_hgrn, page_table,
           g_mix, w_in, lam, g_subln, lb_param, g_rec, w_branch_a, w_branch_r, w_out, g_ffn,
           w_route_group, b_route_group, w_route_expert, b_route_expert, w_exp_gate, w_exp_up,
           w_exp_down, g_ple, w_ple_gate, w_ple, g_final):
    return _run((x_prompt, x_sample, p_prompt, p_sample, cache_k, cache_v, state_hgrn, page_table,
                 g_mix, w_in, lam, g_subln, lb_param, g_rec, w_branch_a, w_branch_r, w_out, g_ffn,
                 w_route_group, b_route_group, w_route_expert, b_route_expert, w_exp_gate, w_exp_up,
                 w_exp_down, g_ple, w_ple_gate, w_ple, g_final), 8)
```
